# Optimizing a Trainium2 kernel written in Bass

```python
import jax, jax.numpy as jnp
from jax import lax
import numpy as np

D_MODEL = 1024
BATCH = 32
SEQ = 2048
DEPTH = 2

GRID_W = 64
CTX_LEN = 256
HEAD_DIM = 64
A_HEADS = 6
A_KV_HEADS = 2
A_GROUP = A_HEADS // A_KV_HEADS
B_HEADS = 5
NA_WIN_R = 8
NA_WIN_C = 16
C_HEADS = 5
C_Q_RANK = 256
C_KV_RANK = 128
C_NOPE = 64
C_ROPE = 32
C_V = 64
MIX_WIDTH = (A_HEADS + B_HEADS) * HEAD_DIM + C_HEADS * C_V
P_A = (A_HEADS + 2 * A_KV_HEADS) * HEAD_DIM
P_B = 3 * B_HEADS * HEAD_DIM
P_C = C_Q_RANK + C_KV_RANK + C_ROPE
P_IN = P_A + P_B + P_C
Q_BLOCK = 128
ROPE_THETA = 10000.0
N_GROUPS = 4
EXPERTS_PER_GROUP = 8
N_EXPERTS = N_GROUPS * EXPERTS_PER_GROUP
TOP_K = 2
D_EXPERT = 256
ALPHA = (2 * DEPTH) ** 0.25
BETA = (8 * DEPTH) ** -0.25
EPS = 1e-6
N_MOD = 6

kernel_name = "hybrid_dit_gqa_na_mla_hmoe"


def _ln(x):
    xf = x.astype(jnp.float32)
    mu = jnp.mean(xf, -1, keepdims=True)
    var = jnp.mean(jnp.square(xf - mu), -1, keepdims=True)
    return ((xf - mu) * lax.rsqrt(var + EPS)).astype(x.dtype)


def _ln_affine(x, g, b):
    return _ln(x) * g + b


def _rms(x, g):
    xf = x.astype(jnp.float32)
    y = xf * lax.rsqrt(jnp.mean(jnp.square(xf), -1, keepdims=True) + EPS)
    return y.astype(x.dtype) * g


def _axial_rope(n_tok, dim):
    t = jnp.arange(n_tok, dtype=jnp.int32)
    row = (t // GRID_W).astype(jnp.float32)
    col = (t % GRID_W).astype(jnp.float32)
    n_freq = dim // 4
    inv = ROPE_THETA ** (-jnp.arange(n_freq, dtype=jnp.float32) / n_freq)
    ang = jnp.concatenate([row[:, None] * inv, col[:, None] * inv], -1)
    return jnp.cos(ang), jnp.sin(ang)


def _apply_rope(x, cos, sin):
    shape = (1, x.shape[1]) + (1,) * (x.ndim - 3) + (cos.shape[-1],)
    cos = cos.reshape(shape)
    sin = sin.reshape(shape)
    x1, x2 = jnp.split(x.astype(jnp.float32), 2, -1)
    return jnp.concatenate([x1 * cos - x2 * sin, x1 * sin + x2 * cos], -1).astype(x.dtype)


def _latent_attention(q, k, v, kc, vc, scale):
    B, S = q.shape[:2]
    nb = S // Q_BLOCK
    k_all = jnp.concatenate([kc, k], 1)
    v_all = jnp.concatenate([vc, v], 1)
    qb = q.reshape((B, nb, Q_BLOCK) + q.shape[2:]).swapaxes(0, 1)

    def one_block(q_blk):
        s = jnp.einsum('bqkgd,bskd->bkgqs', q_blk, k_all).astype(jnp.float32) * scale
        prob = jax.nn.softmax(s, -1).astype(v.dtype)
        return jnp.einsum('bkgqs,bskd->bqkgd', prob, v_all)

    out = lax.map(one_block, qb)
    return out.swapaxes(0, 1).reshape(B, S, -1)


def _context_attention(q, k, v, scale):
    B, L = q.shape[:2]
    s = jnp.einsum('blkgd,bmkd->bkglm', q, k).astype(jnp.float32) * scale
    prob = jax.nn.softmax(s, -1).astype(v.dtype)
    return jnp.einsum('bkglm,bmkd->blkgd', prob, v).reshape(B, L, -1)


def _neighbourhood_attention(q, k, v, kc, vc, rpb, scale):
    B, S, H, d = q.shape
    rows = S // GRID_W
    win_r = min(NA_WIN_R, rows)
    qg = q.reshape(B, rows, GRID_W, H, d)
    kg = k.reshape(B, rows, GRID_W, H, d)
    vg = v.reshape(B, rows, GRID_W, H, d)
    col = jnp.arange(GRID_W, dtype=jnp.int32)
    c0 = jnp.clip(col - NA_WIN_C // 2, 0, GRID_W - NA_WIN_C)
    col_in = (col[None, :] >= c0[:, None]) & (col[None, :] < c0[:, None] + NA_WIN_C)
    dc = jnp.clip(col[None, :] - col[:, None] + (NA_WIN_C - 1), 0, 2 * NA_WIN_C - 2)
    rpb_c = rpb[:, :, dc]
    n_loc = win_r * GRID_W

    def one_row(r):
        r0 = jnp.clip(r - win_r // 2, 0, rows - win_r)
        q_r = lax.dynamic_index_in_dim(qg, r, axis=1, keepdims=False)
        k_r = lax.dynamic_slice_in_dim(kg, r0, win_r, axis=1)
        v_r = lax.dynamic_slice_in_dim(vg, r0, win_r, axis=1)
        dr = r0 + jnp.arange(win_r, dtype=jnp.int32) - r + (NA_WIN_R - 1)
        bias = jnp.take(rpb_c, dr, axis=1).transpose(0, 2, 1, 3)
        s_loc = jnp.einsum('bqhd,bikhd->bhqik', q_r, k_r).astype(jnp.float32) * scale + bias
        s_loc = jnp.where(col_in[:, None, :], s_loc, -jnp.inf)
        s_ctx = jnp.einsum('bqhd,blhd->bhql', q_r, kc).astype(jnp.float32) * scale
        s = jnp.concatenate([s_loc.reshape(B, H, GRID_W, n_loc), s_ctx], -1)
        prob = jax.nn.softmax(s, -1).astype(v.dtype)
        p_loc = prob[..., :n_loc].reshape(B, H, GRID_W, win_r, GRID_W)
        p_ctx = prob[..., n_loc:]
        return (jnp.einsum('bhqik,bikhd->bqhd', p_loc, v_r)
                + jnp.einsum('bhql,blhd->bqhd', p_ctx, vc))

    out = lax.map(one_row, jnp.arange(rows, dtype=jnp.int32))
    return out.transpose(1, 0, 2, 3, 4).reshape(B, S, H * d)


def _mixer_gqa(p, p_ctx, q_gain, k_gain, cos, sin, with_ctx):
    def heads(t):
        q, k, v = jnp.split(t, [A_HEADS * HEAD_DIM, (A_HEADS + A_KV_HEADS) * HEAD_DIM], -1)
        lead = t.shape[:2]
        q = _rms(q.reshape(lead + (A_KV_HEADS, A_GROUP, HEAD_DIM)), q_gain)
        k = _rms(k.reshape(lead + (A_KV_HEADS, HEAD_DIM)), k_gain)
        return q, k, v.reshape(lead + (A_KV_HEADS, HEAD_DIM))

    q, k, v = heads(p)
    qc, kc, vc = heads(p_ctx)
    q = _apply_rope(q, cos, sin)
    k = _apply_rope(k, cos, sin)
    scale = HEAD_DIM ** -0.5
    y = _latent_attention(q, k, v, kc, vc, scale)
    y_ctx = _context_attention(qc, kc, vc, scale) if with_ctx else None
    return y, y_ctx


def _mixer_na(p, p_ctx, rpb, with_ctx):
    def heads(t):
        lead = t.shape[:2]
        return [u.reshape(lead + (B_HEADS, HEAD_DIM)) for u in jnp.split(t, 3, -1)]

    q, k, v = heads(p)
    qc, kc, vc = heads(p_ctx)
    scale = HEAD_DIM ** -0.5
    y = _neighbourhood_attention(q, k, v, kc, vc, rpb, scale)
    y_ctx = _context_attention(qc[:, :, :, None, :], kc, vc, scale) if with_ctx else None
    return y, y_ctx


def _mixer_mla(p, p_ctx, q_lat_gain, kv_lat_gain, w_uq, w_ukv, cos, sin, with_ctx):
    def project(t):
        cq, ckv, kr = jnp.split(t, [C_Q_RANK, C_Q_RANK + C_KV_RANK], -1)
        lead = t.shape[:2]
        q = (_rms(cq, q_lat_gain) @ w_uq).reshape(lead + (C_HEADS, 1, C_NOPE + C_ROPE))
        kv = (_rms(ckv, kv_lat_gain) @ w_ukv).reshape(lead + (C_HEADS, C_NOPE + C_V))
        k_nope, v = jnp.split(kv, [C_NOPE], -1)
        return q, k_nope, kr[:, :, None, :], v

    def keys(k_nope, kr):
        return jnp.concatenate([k_nope, jnp.broadcast_to(kr, k_nope.shape[:-1] + (C_ROPE,))], -1)

    q, k_nope, kr, v = project(p)
    qc, kc_nope, krc, vc = project(p_ctx)
    q = jnp.concatenate([q[..., :C_NOPE], _apply_rope(q[..., C_NOPE:], cos, sin)], -1)
    k = keys(k_nope, _apply_rope(kr, cos, sin))
    kc = keys(kc_nope, krc)
    scale = (C_NOPE + C_ROPE) ** -0.5
    y = _latent_attention(q, k, v, kc, vc, scale)
    y_ctx = _context_attention(qc, kc, vc, scale) if with_ctx else None
    return y, y_ctx


def _hier_moe(h, w_rg, b_rg, w_re, b_re, w1, w3, w2):
    g_logit = (h @ w_rg).astype(jnp.float32) + b_rg
    g_prob = jax.nn.softmax(g_logit, -1)
    g_idx = jnp.argmax(g_logit, -1)
    g_w = jnp.take_along_axis(g_prob, g_idx[..., None], -1)
    e_logit = ((h @ w_re).astype(jnp.float32) + b_re).reshape(h.shape[:-1] + (N_GROUPS, EXPERTS_PER_GROUP))
    e_in_group = jnp.take_along_axis(e_logit, g_idx[..., None, None], -2)[..., 0, :]
    top_v, top_i = lax.top_k(e_in_group, TOP_K)
    top_w = jax.nn.softmax(top_v, -1) * g_w
    expert_id = g_idx[..., None] * EXPERTS_PER_GROUP + top_i
    combine = jnp.sum(jax.nn.one_hot(expert_id, N_EXPERTS, dtype=jnp.float32) * top_w[..., None], -2).astype(h.dtype)
    out = jnp.zeros_like(h)
    for e in range(N_EXPERTS):
        hidden = jax.nn.silu(h @ w1[e]) * (h @ w3[e])
        out = out + combine[..., e:e + 1] * (hidden @ w2[e])
    return out


def setup_inputs(seed: int = 0) -> dict:
    key = jax.random.key(seed)
    ks = iter(jax.random.split(key, 32))

    def nrm(shape, scale):
        return jax.random.normal(next(ks), shape, jnp.float32) * scale

    L, D = DEPTH, D_MODEL
    return {
        "x": nrm((BATCH, SEQ, D), 1.0),
        "c": nrm((BATCH, D), 1.0),
        "ctx": nrm((BATCH, CTX_LEN, D), 1.0),
        "c_ctx": nrm((D,), 1.0),
        "w_mod": nrm((L, D, N_MOD * D), 0.5 * D ** -0.5),
        "b_mod": nrm((L, N_MOD * D), 0.01),
        "w_in": nrm((L, D, P_IN), D ** -0.5),
        "q_gain_a": 1.0 + nrm((L, HEAD_DIM), 0.01),
        "k_gain_a": 1.0 + nrm((L, HEAD_DIM), 0.01),
        "rpb_b": nrm((L, B_HEADS, 2 * NA_WIN_R - 1, 2 * NA_WIN_C - 1), 0.1),
        "q_lat_gain": 1.0 + nrm((L, C_Q_RANK), 0.01),
        "kv_lat_gain": 1.0 + nrm((L, C_KV_RANK), 0.01),
        "w_uq": nrm((L, C_Q_RANK, C_HEADS * (C_NOPE + C_ROPE)), C_Q_RANK ** -0.5),
        "w_ukv": nrm((L, C_KV_RANK, C_HEADS * (C_NOPE + C_V)), C_KV_RANK ** -0.5),
        "w_out": nrm((L, MIX_WIDTH, D), BETA * MIX_WIDTH ** -0.5),
        "ln1_g": 1.0 + nrm((L, D), 0.01),
        "ln1_b": nrm((L, D), 0.01),
        "w_rg": nrm((L, D, N_GROUPS), D ** -0.5),
        "b_rg": nrm((L, N_GROUPS), 0.01),
        "w_re": nrm((L, D, N_EXPERTS), D ** -0.5),
        "b_re": nrm((L, N_EXPERTS), 0.01),
        "w1": nrm((L, N_EXPERTS, D, D_EXPERT), D ** -0.5),
        "w3": nrm((L, N_EXPERTS, D, D_EXPERT), D ** -0.5),
        "w2": nrm((L, N_EXPERTS, D_EXPERT, D), BETA * D_EXPERT ** -0.5),
        "ln2_g": 1.0 + nrm((L, D), 0.01),
        "ln2_b": nrm((L, D), 0.01),
    }


def reference(x, c, ctx, c_ctx, w_mod, b_mod, w_in, q_gain_a, k_gain_a, rpb_b, q_lat_gain, kv_lat_gain,
              w_uq, w_ukv, w_out, ln1_g, ln1_b, w_rg, b_rg, w_re, b_re, w1, w3, w2, ln2_g, ln2_b):
    B, n_tok = x.shape[:2]
    cos_a, sin_a = _axial_rope(n_tok, HEAD_DIM)
    cos_c, sin_c = _axial_rope(n_tok, C_ROPE)
    xc = ctx
    for l in range(DEPTH):
        with_ctx = l < DEPTH - 1
        mod = (jax.nn.silu(c) @ w_mod[l] + b_mod[l]).reshape(B, N_MOD, 1, D_MODEL)
        mod_c = (jax.nn.silu(c_ctx) @ w_mod[l] + b_mod[l]).reshape(N_MOD, D_MODEL)
        sh1, sc1, g1, sh2, sc2, g2 = (mod[:, i] for i in range(N_MOD))
        sh1c, sc1c, g1c, sh2c, sc2c, g2c = (mod_c[i] for i in range(N_MOD))

        h = _ln(x) * (1 + sc1) + sh1
        hc = _ln(xc) * (1 + sc1c) + sh1c
        pa, pb, pc = jnp.split(h @ w_in[l], [P_A, P_A + P_B], -1)
        pa_c, pb_c, pc_c = jnp.split(hc @ w_in[l], [P_A, P_A + P_B], -1)
        y_gqa, y_gqa_c = _mixer_gqa(pa, pa_c, q_gain_a[l], k_gain_a[l], cos_a, sin_a, with_ctx)
        y_na, y_na_c = _mixer_na(pb, pb_c, rpb_b[l], with_ctx)
        y_mla, y_mla_c = _mixer_mla(pc, pc_c, q_lat_gain[l], kv_lat_gain[l], w_uq[l], w_ukv[l],
                                    cos_c, sin_c, with_ctx)
        y = jnp.concatenate([y_gqa, y_na, y_mla], -1) @ w_out[l]
        x = _ln_affine(ALPHA * x + g1 * y, ln1_g[l], ln1_b[l])

        h = _ln(x) * (1 + sc2) + sh2
        x = _ln_affine(ALPHA * x + g2 * _hier_moe(h, w_rg[l], b_rg[l], w_re[l], b_re[l], w1[l], w3[l], w2[l]),
                       ln2_g[l], ln2_b[l])

        if with_ctx:
            y_c = jnp.concatenate([y_gqa_c, y_na_c, y_mla_c], -1) @ w_out[l]
            xc = _ln_affine(ALPHA * xc + g1c * y_c, ln1_g[l], ln1_b[l])
            hc = _ln(xc) * (1 + sc2c) + sh2c
            xc = _ln_affine(ALPHA * xc + g2c * _hier_moe(hc, w_rg[l], b_rg[l], w_re[l], b_re[l], w1[l], w3[l], w2[l]),
                            ln2_g[l], ln2_b[l])
    return x
```

```python
import os
from contextlib import ExitStack
import numpy as np
import concourse.bass as bass
import concourse.mybir as mybir
from concourse.bass_utils import run_bass_kernel_spmd

F32 = mybir.dt.float32
BF16 = mybir.dt.bfloat16
AF = mybir.ActivationFunctionType
ALU = mybir.AluOpType
AX = mybir.AxisListType

D = 1024
SEQ = 2048
CTX = 256
T = 18
TC = 2
NCORES = 8
NB = 4
L = 2
GRID_W = 64
HD = 64
A_HEADS, A_KV, A_G = 6, 2, 3
B_HEADS = 5
C_HEADS = 5
C_QR, C_KVR, C_NOPE, C_ROPE, C_V = 256, 128, 64, 32, 64
P_A, P_B, P_C = 640, 960, 416
NE = 32
DE = 256
ALPHA = float((2 * L) ** 0.25)
EPS = 1e-6
NMOD = 6
WBLK = 448
NPASS_IN = 6


class Dep:
    __slots__ = ("lw", "rd")

    def __init__(self):
        self.lw = None
        self.rd = {}


class Fw:
    def __init__(self, nc, es, ndma=None):
        self.nc = nc
        self.eng = {}
        ndma = ndma or {"sp": 8, "pool": 8, "act": 4}
        for name, h in [("pe", nc.tensor), ("act", nc.scalar), ("dve", nc.vector), ("pool", nc.gpsimd),
                        ("sp", nc.sync)]:
            self.eng[name] = dict(h=h, sem=es.enter_context(nc.semaphore("s_" + name)), count=0, seen={})
        self.dq = {}
        for q, n in ndma.items():
            keys = []
            for i in range(n):
                key = "d_%s%d" % (q, i)
                self.eng[key] = dict(h=None, sem=es.enter_context(nc.semaphore(key)), count=0, seen={})
                keys.append(key)
            self.dq[q] = dict(keys=keys, nxt=0)
        self.nwait = 0
        self.nins = 0
        self.dead = False

    def _waits(self, eng, reads, writes):
        E = self.eng[eng]
        deps = {}
        for d in reads:
            if d.lw is not None:
                deps[d.lw[0]] = max(deps.get(d.lw[0], 0), d.lw[1])
        for d in writes:
            if d.lw is not None and d.lw[0] != eng:
                deps[d.lw[0]] = max(deps.get(d.lw[0], 0), d.lw[1])
            for e, c in d.rd.items():
                if e != eng:
                    deps[e] = max(deps.get(e, 0), c)
        for e, c in deps.items():
            if e == eng and eng == "pe":
                continue
            if E["seen"].get(e, 0) < c:
                E["h"].wait_ge(self.eng[e]["sem"], c)
                E["seen"][e] = c
                self.nwait += 1

    def op(self, eng, fn, reads=(), writes=(), inc=True):
        if self.dead:
            return None
        E = self.eng[eng]
        self._waits(eng, reads, writes)
        ins = fn(E["h"])
        self.nins += 1
        c = E["count"] + 1
        if inc:
            ins.then_inc(E["sem"], 1)
            E["count"] = c
        for d in reads:
            d.rd[eng] = max(d.rd.get(eng, 0), c)
        for d in writes:
            d.lw = (eng, c)
            d.rd = {}
        return ins

    def dma(self, q, out, in_, reads=(), writes=(), **kw):
        if self.dead:
            return None
        E = self.eng[q]
        self._waits(q, reads, writes)
        Q = self.dq[q]
        key = Q["keys"][Q["nxt"] % len(Q["keys"])]
        Q["nxt"] += 1
        Dm = self.eng[key]
        if E["seen"].get(key, 0) < Dm["count"]:
            E["h"].wait_ge(Dm["sem"], Dm["count"])
            E["seen"][key] = Dm["count"]
        if q == "pool":
            kw.setdefault("max_dma_last_dim", 4096)
        ins = E["h"].dma_start(out=out, in_=in_, **kw)
        ins.then_inc(Dm["sem"], 16)
        Dm["count"] += 16
        self.nins += 1
        for d in reads:
            d.rd[key] = Dm["count"]
        for d in writes:
            d.lw = (key, Dm["count"])
            d.rd = {}
        return ins

    def barrier(self):
        if self.dead:
            return
        for name in ("pe", "act", "dve", "pool", "sp"):
            E = self.eng[name]
            for k, K in self.eng.items():
                if k == name:
                    continue
                if E["seen"].get(k, 0) < K["count"]:
                    E["h"].wait_ge(K["sem"], K["count"])
                    E["seen"][k] = K["count"]

    def final_wait(self, eng="sp"):
        E = self.eng[eng]
        for k, K in self.eng.items():
            if k == eng:
                continue
            if K["count"] > 0 and E["seen"].get(k, 0) < K["count"]:
                E["h"].wait_ge(K["sem"], K["count"])
                E["seen"][k] = K["count"]


def _rope_tables(dim):
    n = np.arange(SEQ, dtype=np.int32)
    row = (n // GRID_W).astype(np.float32)
    col = (n % GRID_W).astype(np.float32)
    nf = dim // 4
    inv = (np.float32(10000.0) ** (-np.arange(nf, dtype=np.float32) / np.float32(nf))).astype(np.float32)
    ang = np.concatenate([row[:, None] * inv, col[:, None] * inv], -1).astype(np.float32)
    cos = np.concatenate([np.ones((CTX, dim // 2), np.float32), np.cos(ang).astype(np.float32)], 0)
    sin = np.concatenate([np.zeros((CTX, dim // 2), np.float32), np.sin(ang).astype(np.float32)], 0)

    def lay(a):
        return np.ascontiguousarray(a.reshape(T, 128, dim // 2).transpose(1, 0, 2))

    return lay(cos), lay(sin)


def _na_plan():
    rows = SEQ // GRID_W
    win_r = min(8, rows)
    ntile = rows // 2
    col = np.arange(GRID_W)
    c0 = np.clip(col - 8, 0, GRID_W - 16)
    pats = {}
    plan = []
    pat_list = []
    for tq in range(ntile):
        ent = []
        r0s = [int(np.clip(2 * tq + a - win_r // 2, 0, rows - win_r)) for a in (0, 1)]
        jmin = min(r0s) // 2
        jmax = (max(r0s) + win_r - 1) // 2
        for j in range(jmin, jmax + 1):
            sig = (j - tq, r0s[0] - 2 * tq, r0s[1] - 2 * tq - 1)
            if sig not in pats:
                dr_idx = np.zeros((128, 128), np.int64)
                dc_idx = np.zeros((128, 128), np.int64)
                msk = np.zeros((128, 128), bool)
                for a in (0, 1):
                    r = 2 * tq + a
                    for b in (0, 1):
                        kr = 2 * j + b
                        row_in = (r0s[a] <= kr) and (kr < r0s[a] + win_r)
                        dr = kr - r + 7
                        for c in range(GRID_W):
                            kc = np.arange(GRID_W)
                            cin = (kc >= c0[c]) & (kc < c0[c] + 16)
                            dc = np.clip(kc - c + 15, 0, 30)
                            p = b * 64 + kc
                            q = a * 64 + c
                            msk[p, q] = cin & row_in
                            dr_idx[p, q] = min(max(dr, 0), 14)
                            dc_idx[p, q] = dc
                pats[sig] = len(pat_list)
                pat_list.append((dr_idx, dc_idx, msk))
            ent.append((j, pats[sig]))
        plan.append(ent)
    return plan, pat_list


_NA_PLAN, _NA_PATS = _na_plan()
NPAT = len(_NA_PATS)


def build_program(nb_run=NB, n_layers=L, dbg=None):
    nc = bass.Bass("TRN2", target_bir_lowering=False)
    es = ExitStack()

    def din(name, shape, dt=F32):
        return nc.dram_tensor(name, list(shape), dt, kind="ExternalInput").ap()

    x_in = din("x", [NB, SEQ, D])
    ctx_in = din("ctx", [NB, CTX, D])
    cT_in = din("cT", [128, 8, NB + 1])
    w_mod = din("w_mod", [L, D, NMOD * D])
    b_mod = din("b_mod", [L, NMOD * D])
    w_inb = din("w_inb", [L, NPASS_IN, D, WBLK])
    gA_in = din("gA", [L, 384])
    gC_in = din("gC", [L, 384])
    w_uq = din("w_uq", [L, C_QR, C_HEADS * 96])
    w_ukv = din("w_ukv", [L, C_KVR, C_HEADS * 128])
    w_out = din("w_out", [L, D, D])
    ln1_g = din("ln1_g", [L, D])
    ln1_b = din("ln1_b", [L, D])
    ln2_g = din("ln2_g", [L, D])
    ln2_b = din("ln2_b", [L, D])
    w_r = din("w_r", [L, D, 36])
    b_r = din("b_r", [L, 36])
    w1 = din("w1", [L, NE, D, DE])
    w3 = din("w3", [L, NE, D, DE])
    w2 = din("w2", [L, NE, DE, D])
    nab = din("nab", [L, B_HEADS, 128, NPAT, 128])
    namask = din("namask", [128, NPAT, 128])
    cosA_in = din("cosA", [128, T, 32])
    sinA_in = din("sinA", [128, T, 32])
    cosC_in = din("cosC", [128, T, 16])
    sinC_in = din("sinC", [128, T, 16])
    ident_in = din("ident", [128, 128])
    out = nc.dram_tensor("out", [NB, SEQ, D], F32, kind="ExternalOutput").ap()
    dbgx = nc.dram_tensor("dbgx", [T, 128, D], F32, kind="ExternalOutput").ap() if dbg is not None else None

    class _Stop(Exception):
        pass

    subcnt = [0]

    def sub(k):
        subcnt[0] += 1
        hit = (dbg == ("sub", k)) or (dbg is not None and dbg[0] == "subn" and dbg[1] == subcnt[0])
        if hit and not fw.dead:
            fw.barrier()
            dbg_dump()
            fw.dead = True

    def chk(k):
        if dbg == ("step", k):
            fw.barrier()
            dbg_dump()
            return True
        return False

    def dbg_dump():
        for t in range(T):
            fw.dma("sp", dbgx[t], x_sb[:, t, :], reads=[xdep[t]])
    _sk = dict(kind="ExternalOutput") if "SCR_INT" not in os.environ else {}
    mod_d = nc.dram_tensor("mod_d", [L, NB + 1, NMOD * D], F32, **_sk).ap()
    hT_d = nc.dram_tensor("hT_d", [T, 128, 8 * 128], BF16, **_sk).ap()

    fw = Fw(nc, es)

    def sb(name, shape, dt=F32):
        return es.enter_context(nc.sbuf_tensor(name, list(shape), dt))

    def ps(name, shape, dt=F32):
        return es.enter_context(nc.psum_tensor(name, list(shape), dt))

    x_sb = sb("x_sb", [128, T, D])
    xdep = [Dep() for _ in range(T)]
    ident_f = sb("ident_f", [128, 128])
    ident_b = sb("ident_b", [128, 128], BF16)
    modT = sb("modT", [128, L, NMOD * 8, NB + 1])
    d_modT = Dep()
    d_ident = Dep()
    d_modd = Dep()
    stat = sb("stat", [128, 4, 16])
    d_stat = [Dep() for _ in range(4)]
    zb = [sb("zb%d" % i, [128, D], BF16) for i in range(2)]
    d_zb = [Dep() for _ in range(2)]
    z32 = [sb("z32_%d" % i, [128, D]) for i in range(2)]
    d_z32 = [Dep() for _ in range(2)]
    lnp = sb("lnp", [128, 2, D])
    d_lnp = Dep()
    gbc = sb("gbc", [128, 2, D])
    d_gbc = Dep()

    pbank = [ps("pb%d" % i, [128, 512]) for i in range(8)]
    d_pb = [Dep() for _ in range(8)]

    def bank_bf16(i):
        return pbank[i][:].bitcast(BF16)

    state = dict(stat=0, z=0)

    fw.dma("sp", ident_f[:], ident_in[:, :], writes=[d_ident])
    fw.op("dve", lambda e: e.tensor_copy(out=ident_b[:], in_=ident_f[:]), reads=[d_ident], writes=[d_ident])

    with ExitStack() as pes:
        def psb(name, shape, dt=F32):
            return pes.enter_context(nc.sbuf_tensor(name + "_pre", list(shape), dt))

        PRE = dbg[1] if (dbg is not None and dbg[0] == "pre") else 99
        cT = psb("cT", [128, 8, NB + 1])
        scT = psb("scT", [128, 8, NB + 1])
        tmpc = psb("tmpc", [128, 8, NB + 1])
        wm = [psb("wm%d" % i, [128, 8, 512]) for i in range(2)]
        d_wm = [Dep() for _ in range(2)]
        mrow = psb("mrow", [NB + 1, NMOD * D])
        bmr = psb("bmr", [NB + 1, NMOD * D])
        d_c, d_mrow, d_bmr = Dep(), Dep(), Dep()
        fw.dma("sp", cT[:], cT_in[:, :, :], writes=[d_c])
        fw.op("act", lambda e: e.activation(out=tmpc[:], in_=cT[:], func=AF.Exp, scale=-1.0), reads=[d_c], writes=[d_c])
        fw.op("dve", lambda e: e.tensor_scalar_add(out=tmpc[:], in0=tmpc[:], scalar1=1.0), reads=[d_c], writes=[d_c])
        fw.op("dve", lambda e: e.reciprocal(out=tmpc[:], in_=tmpc[:]), reads=[d_c], writes=[d_c])
        fw.op("dve", lambda e: e.tensor_tensor(out=scT[:], in0=cT[:], in1=tmpc[:], op=ALU.mult), reads=[d_c], writes=[d_c])
        nblk = NMOD * D // 512
        for l in range(n_layers if PRE >= 3 else 0):
            fw.dma("sp", bmr[:], b_mod[l, :].partition_broadcast(NB + 1), writes=[d_bmr])
            for blk in range(nblk):
                i = blk % 2
                fw.dma("sp", wm[i][:], w_mod[l].rearrange("(k p) n -> p k n", p=128)[:, :, blk * 512:(blk + 1) * 512],
                       writes=[d_wm[i]])
                for k in range(8):
                    fw.op("pe", lambda e: e.matmul(pbank[0][0:NB + 1, :], lhsT=scT[:, k, :], rhs=wm[i][:, k, :],
                                                   start=(k == 0), stop=(k == 7)),
                          reads=[d_c, d_wm[i]], writes=[d_pb[0]], inc=(k == 7))
                fw.op("dve", lambda e: e.tensor_tensor(out=mrow[:, blk * 512:(blk + 1) * 512], in0=pbank[0][0:NB + 1, :],
                                                       in1=bmr[:, blk * 512:(blk + 1) * 512], op=ALU.add),
                      reads=[d_pb[0], d_bmr], writes=[d_mrow])
            if PRE < 4:
                continue
            fw.dma("sp", mod_d[l], mrow[:], reads=[d_mrow], writes=[d_modd])
            if PRE < 5:
                continue
            for c in range(NMOD * 8):
                fw.op("pe", lambda e: e.transpose(pbank[1][:, c * 8:c * 8 + NB + 1], mrow[:, c * 128:(c + 1) * 128],
                                                  ident_f[0:NB + 1, 0:NB + 1]),
                      reads=[d_mrow, d_ident], writes=[d_pb[1]], inc=(c == NMOD * 8 - 1))
            fw.op("dve", lambda e: e.tensor_copy(out=modT[:, l, :, :],
                                                 in_=pbank[1][:, 0:NMOD * 8 * 8].rearrange("p (c e) -> p c e", e=8)[:, :, 0:NB + 1]),
                  reads=[d_pb[1]], writes=[d_modT])
            if PRE < 6:
                continue
            for j in (1, 4):
                fw.op("dve", lambda e: e.tensor_scalar_add(out=modT[:, l, j * 8:(j + 1) * 8, :],
                                                           in0=modT[:, l, j * 8:(j + 1) * 8, :], scalar1=1.0),
                      reads=[d_modT], writes=[d_modT])
        fw.barrier()

    def ln_stats(xt_ap, dx, eps_col=0):
        s = state["stat"] % 4
        state["stat"] += 1
        st = stat[:, s, :]
        dep = d_stat[s]
        fw.op("dve", lambda e: e.bn_stats(out=st[:, 0:6], in_=xt_ap[:, 0:512]), reads=[dx], writes=[dep])
        fw.op("dve", lambda e: e.bn_stats(out=st[:, 6:12], in_=xt_ap[:, 512:1024]), reads=[dx], writes=[dep])
        fw.op("dve", lambda e: e.bn_aggr(out=st[:, 12:14], in_=st[:, 0:12]), reads=[dep], writes=[dep])
        fw.op("act", lambda e: e.activation(out=st[:, 14:15], in_=st[:, 13:14], func=AF.Ln, bias=epsb[:, eps_col:eps_col + 1], scale=1.0),
              reads=[dep, d_eps], writes=[dep])
        fw.op("act", lambda e: e.activation(out=st[:, 14:15], in_=st[:, 14:15], func=AF.Exp, scale=-0.5),
              reads=[dep], writes=[dep])
        fw.op("dve", lambda e: e.tensor_scalar(out=st[:, 15:16], in0=st[:, 12:13], scalar1=st[:, 14:15], scalar2=-1.0,
                                                op0=ALU.mult, op1=ALU.mult), reads=[dep], writes=[dep])
        return st[:, 14:15], st[:, 15:16], dep

    epsb = sb("epsb", [128, 4])
    d_eps = Dep()
    fw.op("dve", lambda e: e.memset(epsb[:], EPS), writes=[d_eps])
    fw.op("dve", lambda e: e.memset(epsb[:, 1:2], EPS / (ALPHA * ALPHA)), reads=[d_eps], writes=[d_eps])

    def ln_mod_T(l, b, jsh, jsc, tiles, sink):
        for t in tiles:
            col = NB if t < TC else b
            rstd, nmr, dst = ln_stats(x_sb[:, t, :], xdep[t])
            zi = state["z"] % 2
            state["z"] += 1
            fw.op("act", lambda e: e.activation(out=zb[zi][:], in_=x_sb[:, t, :], func=AF.Identity, scale=rstd, bias=nmr),
                  reads=[xdep[t], dst], writes=[d_zb[zi]])
            trv7 = bank_bf16(7).rearrange("p (c n) -> p c n", n=128)
            trv6 = bank_bf16(6).rearrange("p (c n) -> p c n", n=128)
            for c in (0, 2, 4, 6):
                fw.op("pe", lambda e: e.transpose(trv7[:, c // 2, :], zb[zi][:, c * 128:(c + 1) * 128], ident_b[:]),
                      reads=[d_zb[zi], d_ident], writes=[d_pb[7]], inc=(c == 6))
            for c in (1, 3, 5, 7):
                fw.op("pe", lambda e: e.transpose(trv6[:, c // 2, :], zb[zi][:, c * 128:(c + 1) * 128], ident_b[:]),
                      reads=[d_zb[zi], d_ident], writes=[d_pb[6]], inc=(c == 7))
            dstap, ddep, post = sink(t)
            for c in range(8):
                scl = modT[:, l, jsc * 8 + c, col:col + 1]
                shf = modT[:, l, jsh * 8 + c, col:col + 1]
                if c % 2 == 0:
                    fw.op("act", lambda e: e.activation(out=dstap[:, c, :], in_=trv7[:, c // 2, :], func=AF.Identity,
                                                        scale=scl, bias=shf),
                          reads=[d_pb[7], d_modT], writes=[ddep])
                else:
                    fw.op("dve", lambda e: e.tensor_scalar(out=dstap[:, c, :], in0=trv6[:, c // 2, :], scalar1=scl, scalar2=shf,
                                                           op0=ALU.mult, op1=ALU.add),
                          reads=[d_pb[6], d_modT], writes=[ddep])
            if post is not None:
                post(t)

    def scale_x(tiles):
        for t in tiles:
            fw.op("pool", lambda e: e.tensor_scalar(out=x_sb[:, t, :], in0=x_sb[:, t, :], scalar1=ALPHA, scalar2=None,
                                                    op0=ALU.mult), reads=[xdep[t]], writes=[xdep[t]])

    def ln_affine(l, g_in, b_in, tiles):
        fw.dma("sp", lnp[:, 0, :], g_in[l, :].partition_broadcast(128), writes=[d_lnp])
        fw.dma("sp", lnp[:, 1, :], b_in[l, :].partition_broadcast(128), writes=[d_lnp])
        for t in tiles:
            rstd, nmr, dst = ln_stats(x_sb[:, t, :], xdep[t], eps_col=1)
            zi = state["z"] % 2
            state["z"] += 1
            fw.op("act", lambda e: e.activation(out=z32[zi][:], in_=x_sb[:, t, :], func=AF.Identity, scale=rstd, bias=nmr),
                  reads=[xdep[t], dst], writes=[d_z32[zi]])
            fw.op("dve", lambda e: e.tensor_tensor(out=z32[zi][:], in0=z32[zi][:], in1=lnp[:, 0, :], op=ALU.mult),
                  reads=[d_z32[zi], d_lnp], writes=[d_z32[zi]])
            fw.op("pool", lambda e: e.tensor_tensor(out=x_sb[:, t, :], in0=z32[zi][:], in1=lnp[:, 1, :], op=ALU.add),
                  reads=[d_z32[zi], d_lnp], writes=[xdep[t]])

    def load_gates(l, b, j):
        fw.dma("sp", gbc[:, 0, :], mod_d[l, b, j * D:(j + 1) * D].partition_broadcast(128), reads=[d_modd], writes=[d_gbc])
        fw.dma("sp", gbc[:, 1, :], mod_d[l, NB, j * D:(j + 1) * D].partition_broadcast(128), reads=[d_modd], writes=[d_gbc])
        fw.op("dve", lambda e: e.tensor_scalar(out=gbc[:], in0=gbc[:], scalar1=1.0 / ALPHA, scalar2=None, op0=ALU.mult),
              reads=[d_gbc], writes=[d_gbc])

    def main_loops():
      if dbg is not None and dbg[0] == "pre":
          return
      for b in range(nb_run):
        for t in range(T):
            src = ctx_in[b, t * 128:(t + 1) * 128, :] if t < TC else x_in[b, (t - TC) * 128:(t - TC + 1) * 128, :]
            fw.dma("sp" if t % 2 == 0 else "act", x_sb[:, t, :], src, writes=[xdep[t]])
        if chk(1):
            return
        for l in range(n_layers):
            with_ctx = l < n_layers - 1
            qtiles = list(range(0 if with_ctx else TC, T))
            with ExitStack() as aes:
                def asb(name, shape, dt=F32):
                    return aes.enter_context(nc.sbuf_tensor("%s_a%d_%d" % (name, b, l), list(shape), dt))

                hst = [asb("hst%d" % i, [128, 8, 128], BF16) for i in range(2)]
                d_hst = [Dep() for _ in range(2)]
                d_hTd = [Dep() for _ in range(T)]
                hld = [asb("hld%d" % i, [128, 8, 128], BF16) for i in range(2)]
                d_hld = [Dep() for _ in range(2)]
                wblk = asb("wblk", [128, 8, WBLK], BF16)
                d_wblk = Dep()
                wo = asb("wo", [128, 2, 2, D], BF16)
                d_wo = Dep()
                qkT = asb("qkT", [128, 3, T * 128], BF16)
                d_qkT = Dep()
                vaug = asb("vaug", [128, T, 2, 65], BF16)
                d_v = Dep()
                ypass = asb("ypass", [128, T, 192], BF16)
                d_y = Dep()
                yT = [asb("yT%d" % i, [128, 2, 128], BF16) for i in range(2)]
                d_yT = [Dep() for _ in range(2)]
                pT = [asb("pT%d" % i, [128, 512], BF16) for i in range(3)]
                d_pT = [Dep() for _ in range(3)]
                rcp = [asb("rcp%d" % i, [128, 4]) for i in range(2)]
                d_rcp = [Dep() for _ in range(2)]
                tm = [asb("tm%d" % i, [128, WBLK], BF16) for i in range(2)]
                d_tm = [Dep() for _ in range(2)]
                f1 = asb("f1", [128, 384])
                f2 = asb("f2", [128, 384])
                f3 = asb("f3", [128, 384])
                f4 = asb("f4", [128, 384])
                d_f = Dep()
                d_rp = [Dep() for _ in range(4)]
                ssq = asb("ssq", [128, 8])
                gains = asb("gains", [128, 384])
                d_gains = Dep()
                cosT = asb("cosT", [128, T, 32])
                sinT = asb("sinT", [128, T, 32])
                d_cs = Dep()
                d_lat = Dep()
                d_wh = Dep()
                d_na = Dep()
                d_nas = Dep()
                cnt = dict(h=0, pT=0, sT=0, pv=0, tm=0, yT=0, o=0, rc=0)

                fw.op("pool", lambda e: e.memset(vaug[:], 1.0), writes=[d_v])

                def sink1(t):
                    i = cnt["h"] % 2
                    cnt["h"] += 1

                    def post(t, i=i):
                        fw.dma("sp", hT_d[t].rearrange("p (c n) -> p c n", n=128), hst[i][:], reads=[d_hst[i]], writes=[d_hTd[t]])
                    return hst[i], d_hst[i], post

                ln_mod_T(l, b, 0, 1, range(T), sink1)
                if chk(2):
                    return
                load_gates(l, b, 2)
                if chk(3):
                    return

                def load_wblk(pi):
                    fw.dma("pool", wblk[:], w_inb[l, pi].rearrange("(k p) n -> p k n", p=128), writes=[d_wblk])

                def load_wo(r0, nr):
                    n0 = min(nr, 128)
                    fw.dma("pool", wo[0:n0, 0, 0, :], w_out[l, r0:r0 + n0, :], writes=[d_wo])
                    if nr > 128:
                        fw.dma("pool", wo[0:nr - 128, 0, 1, :], w_out[l, r0 + 128:r0 + nr, :], writes=[d_wo])
                    for kc in range(2 if nr > 128 else 1):
                        n1 = min(nr - kc * 128, 128)
                        if with_ctx:
                            fw.op("pool", lambda e: e.tensor_tensor(out=wo[0:n1, 1, kc, :], in0=wo[0:n1, 0, kc, :],
                                                                    in1=gbc[0:n1, 1, :], op=ALU.mult),
                                  reads=[d_wo, d_gbc], writes=[d_wo])
                        fw.op("pool", lambda e: e.tensor_tensor(out=wo[0:n1, 0, kc, :], in0=wo[0:n1, 0, kc, :],
                                                                in1=gbc[0:n1, 0, :], op=ALU.mult),
                              reads=[d_wo, d_gbc], writes=[d_wo])

                def project(ncols, cb):
                    def mm(t):
                        i = t % 2
                        fw.dma("sp", hld[i][:], hT_d[t].rearrange("p (c n) -> p c n", n=128), reads=[d_hTd[t]], writes=[d_hld[i]])
                        pb = 0 + (t % 2)
                        for k in range(8):
                            fw.op("pe", lambda e: e.matmul(pbank[pb][:, 0:ncols], lhsT=hld[i][:, k, :], rhs=wblk[:, k, 0:ncols],
                                                           start=(k == 0), stop=(k == 7)),
                                  reads=[d_hld[i], d_wblk], writes=[d_pb[pb]], inc=(k == 7))
                        return pb

                    pend = mm(0)
                    for t in range(T):
                        pb = pend
                        if t + 1 < T:
                            pend = mm(t + 1)
                        cb(t, pbank[pb], d_pb[pb])

                def rope(src3, dst3, ng, half, t, cs_half, ddst, cs=None):
                    if cs is not None:
                        cosb, sinb = cs
                    else:
                        cosb = cosT[:, t, 0:cs_half].unsqueeze(1).to_broadcast([128, ng, half])
                        sinb = sinT[:, t, 0:cs_half].unsqueeze(1).to_broadcast([128, ng, half])
                    x1 = src3[:, :, 0:half]
                    x2 = src3[:, :, half:2 * half]
                    a = f3[:, 0:ng * half].rearrange("p (g d) -> p g d", d=half)
                    bq = f3[:, 192:192 + ng * half].rearrange("p (g d) -> p g d", d=half)
                    c = f4[:, 0:ng * half].rearrange("p (g d) -> p g d", d=half)
                    dq = f4[:, 192:192 + ng * half].rearrange("p (g d) -> p g d", d=half)
                    fw.op("dve", lambda e: e.tensor_tensor(out=a, in0=x1, in1=cosb, op=ALU.mult), reads=[d_f, d_cs], writes=[d_rp[0]])
                    fw.op("dve", lambda e: e.tensor_tensor(out=bq, in0=x2, in1=sinb, op=ALU.mult), reads=[d_f, d_cs], writes=[d_rp[1]])
                    fw.op("dve", lambda e: e.tensor_tensor(out=c, in0=x1, in1=sinb, op=ALU.mult), reads=[d_f, d_cs], writes=[d_rp[2]])
                    fw.op("dve", lambda e: e.tensor_tensor(out=dq, in0=x2, in1=cosb, op=ALU.mult), reads=[d_f, d_cs], writes=[d_rp[3]])
                    fw.op("dve", lambda e: e.tensor_tensor(out=dst3[:, :, 0:half], in0=a, in1=bq, op=ALU.subtract),
                          reads=[d_rp[0], d_rp[1]], writes=[ddst])
                    fw.op("dve", lambda e: e.tensor_tensor(out=dst3[:, :, half:2 * half], in0=c, in1=dq, op=ALU.add),
                          reads=[d_rp[2], d_rp[3]], writes=[ddst])

                def rstd_of(ss_ap, n, inv_n):
                    fw.op("act", lambda e: e.activation(out=ss_ap, in_=ss_ap, func=AF.Ln, bias=epsb[:, 0:1], scale=inv_n),
                          reads=[d_f, d_eps], writes=[d_f])
                    fw.op("act", lambda e: e.activation(out=ss_ap, in_=ss_ap, func=AF.Exp, scale=-0.5), reads=[d_f], writes=[d_f])

                def transposes_to(src_tm, src_dep, blocks, dst, dst_dep, t):
                    trv = bank_bf16(7).rearrange("p (c n) -> p c n", n=128)
                    for bi, (c0, ncol, blk) in enumerate(blocks):
                        fw.op("pe", lambda e: e.transpose(trv[0:ncol, bi, :], src_tm[:, c0:c0 + ncol], ident_b[:]),
                              reads=[src_dep, d_ident], writes=[d_pb[7]], inc=(bi == len(blocks) - 1))
                    for bi, (c0, ncol, blk) in enumerate(blocks):
                        p0 = 0
                        fw.op("act", lambda e: e.activation(out=dst[p0:ncol, blk, t * 128:(t + 1) * 128], in_=trv[p0:ncol, bi, :],
                                                            func=AF.Copy),
                              reads=[d_pb[7]], writes=[dst_dep])

                def attention(heads, qblocks, scale):
                    for hd in heads:
                        qp0, K, qblk = hd["q"]
                        kp0, _, kblk = hd["k"]
                        for (qts, kts, bsel) in qblocks:
                            nq = len(qts)
                            n = nq * 128
                            q0 = qts[0] * 128
                            pvb = 4 + (cnt["pv"] % 2)
                            cnt["pv"] += 1
                            pvv = pbank[pvb][:, 0:4 * 65].rearrange("p (j d) -> p j d", d=65)

                            def s_mm(i):
                                kt = kts[i]
                                sb_ = 2 + (cnt["sT"] % 2)
                                cnt["sT"] += 1
                                bias_ap = hd["bias"](bsel, kt) if (hd.get("bias") is not None and bsel is not None) else None
                                fw.op("pe", lambda e: e.matmul(pbank[sb_][:, 0:n], lhsT=qkT[kp0:kp0 + K, kblk, kt * 128:(kt + 1) * 128],
                                                               rhs=qkT[qp0:qp0 + K, qblk, q0:q0 + n], start=True, stop=(bias_ap is None)),
                                      reads=[d_qkT], writes=[d_pb[sb_]], inc=(bias_ap is None))
                                if bias_ap is not None:
                                    fw.op("pe", lambda e: e.matmul(pbank[sb_][:, 0:n], lhsT=ident_b[:], rhs=bias_ap, start=False, stop=True),
                                          reads=[d_na, d_ident], writes=[d_pb[sb_]])
                                return sb_

                            pend = s_mm(0)
                            for i in range(len(kts)):
                                sb_ = pend
                                if i + 1 < len(kts):
                                    pend = s_mm(i + 1)
                                pi = cnt["pT"] % 3
                                cnt["pT"] += 1
                                fw.op("act", lambda e: e.activation(out=pT[pi][:, 0:n], in_=pbank[sb_][:, 0:n], func=AF.Exp, scale=scale),
                                      reads=[d_pb[sb_]], writes=[d_pT[pi]])
                                kt = kts[i]
                                for j in range(nq):
                                    fw.op("pe", lambda e: e.matmul(pvv[:, j, :], lhsT=pT[pi][:, j * 128:(j + 1) * 128],
                                                                   rhs=vaug[:, kt, hd["v"], :], start=(i == 0 and j == 0),
                                                                   stop=(i == len(kts) - 1), skip_group_check=True),
                                          reads=[d_pT[pi], d_v], writes=[d_pb[pvb]], inc=(j == nq - 1))
                            ri = cnt["rc"] % 2
                            cnt["rc"] += 1
                            fw.op("dve", lambda e: e.reciprocal(out=rcp[ri][:, 0:nq], in_=pvv[:, 0:nq, 64]), reads=[d_pb[pvb]],
                                  writes=[d_rcp[ri]])
                            yc = hd["ycol"]
                            fw.op("dve", lambda e: e.tensor_tensor(out=ypass[:, qts[0]:qts[0] + nq, yc:yc + 64], in0=pvv[:, 0:nq, 0:64],
                                                                   in1=rcp[ri][:, 0:nq].unsqueeze(2).to_broadcast([128, nq, 64]),
                                                                   op=ALU.mult),
                                  reads=[d_pb[pvb], d_rcp[ri]], writes=[d_y])

                def out_proj(ncol):
                    chunks = [(0, min(ncol, 128))] + ([(128, ncol - 128)] if ncol > 128 else [])
                    trv = bank_bf16(7).rearrange("p (c n) -> p c n", n=128)
                    tl = list(qtiles)

                    def stage_a(idx):
                        t = tl[idx]
                        yi = cnt["yT"] % 2
                        cnt["yT"] += 1
                        cb = 2 * (idx % 2)
                        for ci, (c0, ncl) in enumerate(chunks):
                            fw.op("pe", lambda e: e.transpose(trv[0:ncl, cb + ci, :], ypass[:, t, c0:c0 + ncl], ident_b[:]),
                                  reads=[d_y, d_ident], writes=[d_pb[7]], inc=(ci == len(chunks) - 1))
                        for ci, (c0, ncl) in enumerate(chunks):
                            fw.op("act", lambda e: e.activation(out=yT[yi][0:ncl, ci, :], in_=trv[0:ncl, cb + ci, :], func=AF.Copy),
                                  reads=[d_pb[7]], writes=[d_yT[yi]])
                        return yi

                    def stage_b(idx, yi):
                        t = tl[idx]
                        ver = 1 if t < TC else 0
                        for half in range(2):
                            ob = 2 * (idx % 2) + half
                            for ci, (c0, ncl) in enumerate(chunks):
                                fw.op("pe", lambda e: e.matmul(pbank[ob][:, :], lhsT=yT[yi][0:ncl, ci, :],
                                                               rhs=wo[0:ncl, ver, ci, half * 512:(half + 1) * 512],
                                                               start=(ci == 0), stop=(ci == len(chunks) - 1)),
                                      reads=[d_yT[yi], d_wo], writes=[d_pb[ob]], inc=(ci == len(chunks) - 1))
                            fw.op("dve", lambda e: e.tensor_tensor(out=x_sb[:, t, half * 512:(half + 1) * 512],
                                                                   in0=x_sb[:, t, half * 512:(half + 1) * 512], in1=pbank[ob][:, :],
                                                                   op=ALU.add),
                                  reads=[d_pb[ob], xdep[t]], writes=[xdep[t]])

                    pend = stage_a(0)
                    for idx in range(len(tl)):
                        yi = pend
                        if idx + 1 < len(tl):
                            pend = stage_a(idx + 1)
                        stage_b(idx, yi)

                lat_blocks = [(list(range(TC + 4 * i, TC + 4 * i + 4)), list(range(T)), None) for i in range(4)]
                ctx_blocks = [([0, 1], [0, 1], None)] if with_ctx else []

                fw.dma("sp", cosT[:], cosA_in[:, :, :], writes=[d_cs])
                fw.dma("sp", sinT[:], sinA_in[:, :, :], writes=[d_cs])
                fw.dma("sp", gains[:], gA_in[l, :].partition_broadcast(128), writes=[d_gains])
                if chk(4):
                    return
                for j in range(A_KV):
                    load_wblk(j)
                    if chk(5):
                        return
                    load_wo(j * 192, 192)
                    if chk(6):
                        return

                    def cbA(t, pb, dpb):
                        ti = cnt["tm"] % 2
                        cnt["tm"] += 1
                        parts = os.environ.get("CB_PARTS", "1234")
                        if "1" in parts:
                            if "NOSQ" in os.environ:
                                fw.op("act", lambda e: e.activation(out=f2[:], in_=pb[:, 0:384], func=AF.Copy), reads=[dpb], writes=[d_f])
                                fw.op("dve", lambda e: e.tensor_tensor(out=f1[:], in0=f2[:], in1=f2[:], op=ALU.mult), reads=[d_f], writes=[d_f])
                            else:
                                fw.op("act", lambda e: e.activation(out=f1[:], in_=pb[:, 0:384], func=AF.Square), reads=[dpb], writes=[d_f])
                            fw.op("dve", lambda e: e.tensor_reduce(out=ssq[:, 0:6], in_=f1[:].rearrange("p (g d) -> p g d", d=64),
                                                                   axis=AX.X, op=ALU.add), reads=[d_f], writes=[d_f])
                            rstd_of(ssq[:, 0:6], 6, 1.0 / 64)
                            fw.op("dve", lambda e: e.tensor_tensor(out=f2[:].rearrange("p (g d) -> p g d", d=64),
                                                                   in0=pb[:, 0:384].rearrange("p (g d) -> p g d", d=64),
                                                                   in1=ssq[:, 0:6].unsqueeze(2).to_broadcast([128, 6, 64]), op=ALU.mult),
                                  reads=[dpb, d_f], writes=[d_f])
                            fw.op("dve", lambda e: e.tensor_tensor(out=f2[:], in0=f2[:], in1=gains[:], op=ALU.mult),
                                  reads=[d_f, d_gains], writes=[d_f])
                        if "2" in parts:
                            rope(f2[:].rearrange("p (g d) -> p g d", d=64), tm[ti][:, 0:384].rearrange("p (g d) -> p g d", d=64), 6, 32, t, 32, d_tm[ti])
                        if "3" in parts:
                            if os.environ.get("V_ENG", "dve") == "act":
                                fw.op("act", lambda e: e.activation(out=vaug[:, t, 0, 0:64], in_=pb[:, 384:448], func=AF.Copy),
                                      reads=[dpb], writes=[d_v])
                            else:
                                fw.op("dve", lambda e: e.tensor_copy(out=vaug[:, t, 0, 0:64], in_=pb[:, 384:448]),
                                      reads=[dpb], writes=[d_v])
                        if "4" in parts:
                            transposes_to(tm[ti], d_tm[ti], [(0, 128, 0), (128, 128, 1), (256, 128, 2)], qkT, d_qkT, t)

                    project(448, cbA)
                    if chk(7):
                        return
                    heads = [dict(q=((g % 2) * 64, 64, g // 2), k=((g % 2) * 64, 64, 2), v=0, ycol=g * 64) for g in range(A_G)]
                    attention(heads, ctx_blocks + lat_blocks, HD ** -0.5)
                    if chk(8):
                        return
                    out_proj(192)
                    if chk(9):
                        return

                if dbg == ("A", l):
                    fw.barrier()
                    dbg_dump()
                    return
                fw.barrier()
                with ExitStack() as bes:
                    natab = bes.enter_context(nc.sbuf_tensor("natab_%d_%d" % (b, l), [128, 2, NPAT, 128], BF16))
                    nmaskb = bes.enter_context(nc.sbuf_tensor("nmaskb_%d_%d" % (b, l), [128, NPAT, 128], BF16))
                    fw.dma("pool", nmaskb[:], namask[:, :, :], writes=[d_nas])
                    for pi in range(3):
                        h0 = 2 * pi
                        h1 = min(2 * pi + 1, B_HEADS - 1)
                        nh = 2 if pi < 2 else 1
                        load_wblk(2 + pi)
                        for hh, hx in enumerate((h0, h1)[:nh]):
                            fw.dma("pool", natab[:, hh, :, :], nab[l, hx], writes=[d_na])
                            fw.op("pool", lambda e: e.tensor_tensor(out=natab[:, hh, :, :], in0=natab[:, hh, :, :], in1=nmaskb[:], op=ALU.add),
                                  reads=[d_nas, d_na], writes=[d_na])
                        load_wo(384 + h0 * 64, nh * 64)

                        def cbB(t, pb, dpb):
                            ti = cnt["tm"] % 2
                            cnt["tm"] += 1
                            fw.op("dve", lambda e: e.tensor_scalar(out=tm[ti][:, 0:128], in0=pb[:, 0:128], scalar1=float(HD ** -0.5),
                                                                   scalar2=None, op0=ALU.mult), reads=[dpb], writes=[d_tm[ti]])
                            fw.op("dve", lambda e: e.tensor_copy(out=tm[ti][:, 128:256], in_=pb[:, 128:256]), reads=[dpb],
                                  writes=[d_tm[ti]])
                            fw.op("dve", lambda e: e.tensor_copy(out=vaug[:, t, :, 0:64], in_=pb[:, 256:384].rearrange("p (g d) -> p g d", d=64)),
                                  reads=[dpb], writes=[d_v])
                            transposes_to(tm[ti], d_tm[ti], [(0, 128, 0), (128, 128, 1)], qkT, d_qkT, t)

                        project(384, cbB)
                        heads = []
                        for hh in range(nh):
                            heads.append(dict(q=(hh * 64, 64, 0), k=(hh * 64, 64, 1), v=hh, ycol=hh * 64,
                                              bias=(lambda bsel, kt, hh=hh: natab[:, hh, bsel[kt], :] if kt in bsel else None)))
                        qb = list(ctx_blocks)
                        for tq in range(T - TC):
                            ent = _NA_PLAN[tq]
                            kts = [TC + jj for (jj, pid) in ent] + [0, 1]
                            bsel = {TC + jj: pid for (jj, pid) in ent}
                            qb.append(([TC + tq], kts, bsel))
                        attention(heads, qb, 1.0)
                        out_proj(nh * 64)
                    fw.barrier()

                if dbg == ("B", l):
                    dbg_dump()
                    return
                with ExitStack() as ces:
                    latT = ces.enter_context(nc.sbuf_tensor("latT_%d_%d" % (b, l), [128, 3, T * 128], BF16))
                    wq_h = ces.enter_context(nc.sbuf_tensor("wq_h_%d_%d" % (b, l), [128, 2, 96], BF16))
                    wkv_h = ces.enter_context(nc.sbuf_tensor("wkv_h_%d_%d" % (b, l), [128, 128], BF16))
                    fw.dma("sp", cosT[:, :, 0:16], cosC_in[:, :, :], writes=[d_cs])
                    fw.dma("sp", sinT[:, :, 0:16], sinC_in[:, :, :], writes=[d_cs])
                    fw.dma("sp", gains[:], gC_in[l, :].partition_broadcast(128), writes=[d_gains])
                    load_wblk(5)

                    def cbC(t, pb, dpb):
                        ti = cnt["tm"] % 2
                        cnt["tm"] += 1
                        fw.op("act", lambda e: e.activation(out=f1[:], in_=pb[:, 0:384], func=AF.Square), reads=[dpb], writes=[d_f])
                        fw.op("dve", lambda e: e.tensor_reduce(out=ssq[:, 0:1], in_=f1[:, 0:256], axis=AX.X, op=ALU.add), reads=[d_f], writes=[d_f])
                        fw.op("dve", lambda e: e.tensor_reduce(out=ssq[:, 1:2], in_=f1[:, 256:384], axis=AX.X, op=ALU.add), reads=[d_f], writes=[d_f])
                        rstd_of(ssq[:, 0:1], 1, 1.0 / 256)
                        rstd_of(ssq[:, 1:2], 1, 1.0 / 128)
                        fw.op("dve", lambda e: e.tensor_scalar(out=f2[:, 0:256], in0=pb[:, 0:256], scalar1=ssq[:, 0:1], scalar2=None, op0=ALU.mult),
                              reads=[dpb, d_f], writes=[d_f])
                        fw.op("dve", lambda e: e.tensor_scalar(out=f2[:, 256:384], in0=pb[:, 256:384], scalar1=ssq[:, 1:2], scalar2=None, op0=ALU.mult),
                              reads=[dpb, d_f], writes=[d_f])
                        fw.op("pool", lambda e: e.tensor_tensor(out=tm[ti][:, 0:384], in0=f2[:], in1=gains[:], op=ALU.mult),
                              reads=[d_f, d_gains], writes=[d_tm[ti]])
                        fw.op("dve", lambda e: e.tensor_copy(out=f1[:, 0:32], in_=pb[:, 384:416]), reads=[dpb], writes=[d_f])
                        rope(f1[:, 0:32].rearrange("p (g d) -> p g d", d=32), tm[ti][:, 384:416].rearrange("p (g d) -> p g d", d=32), 1, 16, t, 16, d_tm[ti])
                        transposes_to(tm[ti], d_tm[ti], [(0, 128, 0), (128, 128, 1), (256, 128, 2)], latT, d_lat, t)
                        trv = bank_bf16(7).rearrange("p (c n) -> p c n", n=128)
                        fw.op("pe", lambda e: e.transpose(trv[0:96, 3, :], tm[ti][:, 320:416], ident_b[:]),
                              reads=[d_tm[ti], d_ident], writes=[d_pb[7]])
                        fw.op("act", lambda e: e.activation(out=qkT[64:96, 1, t * 128:(t + 1) * 128], in_=trv[64:96, 3, :], func=AF.Copy),
                              reads=[d_pb[7]], writes=[d_qkT])

                    project(416, cbC)
                    for h in range(C_HEADS):
                        fw.dma("pool", wq_h[:], w_uq[l].rearrange("(k p) n -> p k n", p=128)[:, :, h * 96:(h + 1) * 96], writes=[d_wh])
                        fw.dma("pool", wkv_h[:], w_ukv[l, :, h * 128:(h + 1) * 128], writes=[d_wh])
                        if h == 0:
                            load_wo(704, 192)
                        if h == 3:
                            load_wo(704 + 192, 128)
                        def mmC2(p):
                            pb = p % 2
                            for s_ in range(2):
                                t = 2 * p + s_
                                o = s_ * 160
                                for kc in range(2):
                                    fw.op("pe", lambda e: e.matmul(pbank[pb][:, o:o + 96], lhsT=latT[:, kc, t * 128:(t + 1) * 128],
                                                                   rhs=wq_h[:, kc, :], start=(kc == 0), stop=(kc == 1)),
                                          reads=[d_lat, d_wh], writes=[d_pb[pb]], inc=False)
                                fw.op("pe", lambda e: e.matmul(pbank[pb][:, o + 96:o + 160], lhsT=latT[:, 2, t * 128:(t + 1) * 128],
                                                               rhs=wkv_h[:, 64:128], start=False, stop=True, skip_group_check=True),
                                      reads=[d_lat, d_wh], writes=[d_pb[pb]], inc=(s_ == 1))
                            return pb

                        pendC = mmC2(0)
                        for p in range(T // 2):
                            ti = cnt["tm"] % 2
                            cnt["tm"] += 1
                            pb = pendC
                            if p + 1 < T // 2:
                                pendC = mmC2(p + 1)
                            t0 = 2 * p
                            pv = pbank[pb][:, 0:320].rearrange("p (s c) -> p s c", c=160)
                            tmv = tm[ti][:, 0:192].rearrange("p (s c) -> p s c", c=96)
                            fw.op("act", lambda e: e.activation(out=tmv[:, :, 0:64], in_=pv[:, :, 0:64], func=AF.Copy), reads=[d_pb[pb]],
                                  writes=[d_tm[ti]])
                            fw.op("act", lambda e: e.activation(out=vaug[:, t0:t0 + 2, 0, 0:64], in_=pv[:, :, 96:160], func=AF.Copy),
                                  reads=[d_pb[pb]], writes=[d_v])
                            fw.op("act", lambda e: e.activation(out=f1[:, 0:64].rearrange("p (s c) -> p s c", c=32), in_=pv[:, :, 64:96],
                                                                func=AF.Copy), reads=[d_pb[pb]], writes=[d_f])
                            rope(f1[:, 0:64].rearrange("p (g d) -> p g d", d=32), tmv[:, :, 64:96], 2, 16, t0, 16, d_tm[ti],
                                 cs=(cosT[:, t0:t0 + 2, 0:16], sinT[:, t0:t0 + 2, 0:16]))
                            trv = bank_bf16(7).rearrange("p (c n) -> p c n", n=128)
                            for s_ in range(2):
                                fw.op("pe", lambda e: e.transpose(trv[0:96, s_, :], tm[ti][:, s_ * 96:(s_ + 1) * 96], ident_b[:]),
                                      reads=[d_tm[ti], d_ident], writes=[d_pb[7]], inc=(s_ == 1))
                            fw.op("act", lambda e: e.activation(out=qkT[0:96, 0, t0 * 128:(t0 + 2) * 128].rearrange("p (s n) -> p s n", n=128),
                                                                in_=trv[0:96, 0:2, :], func=AF.Copy),
                                  reads=[d_pb[7]], writes=[d_qkT])
                        for blk in range(0, T * 128, 512):
                            nn = min(512, T * 128 - blk)
                            sbk = 2 + ((blk // 512) % 2)
                            fw.op("pe", lambda e: e.matmul(pbank[sbk][0:64, 0:nn], lhsT=wkv_h[:, 0:64], rhs=latT[:, 2, blk:blk + nn], start=True, stop=True),
                                  reads=[d_lat, d_wh], writes=[d_pb[sbk]])
                            fw.op("act", lambda e: e.activation(out=qkT[0:64, 1, blk:blk + nn], in_=pbank[sbk][0:64, 0:nn], func=AF.Copy),
                                  reads=[d_pb[sbk]], writes=[d_qkT])
                        heads = [dict(q=(0, 96, 0), k=(0, 96, 1), v=0, ycol=(h % 3) * 64)]
                        attention(heads, ctx_blocks + lat_blocks, (C_NOPE + C_ROPE) ** -0.5)
                        if h == 2:
                            out_proj(192)
                        if h == 4:
                            out_proj(128)

                ln_affine(l, ln1_g, ln1_b, qtiles)
                fw.barrier()
                if dbg == ("attn", l):
                    dbg_dump()
                    return

            with ExitStack() as mes:
                def msb(name, shape, dt=F32):
                    return mes.enter_context(nc.sbuf_tensor("%s_m%d_%d" % (name, b, l), list(shape), dt))

                h2T = msb("h2T", [128, 8, T * 128], BF16)
                d_h2 = [Dep() for _ in range(T)]
                w13 = [msb("w13_%d" % i, [128, 8, 512], BF16) for i in range(2)]
                w2s = [msb("w2s_%d" % i, [128, 2, 2, D], BF16) for i in range(2)]
                d_w13 = [Dep() for _ in range(2)]
                d_w2 = [Dep() for _ in range(2)]
                wr = msb("wr", [128, 8, 36], BF16)
                brb = msb("brb", [128, 36])
                d_wr = Dep()
                comb = msb("comb", [128, T, NE])
                d_comb = [Dep() for _ in range(T)]
                lg = msb("lg", [128, 36])
                rt = msb("rt", [128, 96])
                d_rt = Dep()

                def sink2(t):
                    return h2T[:, :, t * 128:(t + 1) * 128], d_h2[t], None

                ln_mod_T(l, b, 3, 4, qtiles, sink2)
                load_gates(l, b, 5)
                fw.dma("pool", wr[:], w_r[l].rearrange("(k p) n -> p k n", p=128), writes=[d_wr])
                fw.dma("sp", brb[:], b_r[l, :].partition_broadcast(128), writes=[d_wr])
                for t in qtiles:
                    for k in range(8):
                        fw.op("pe", lambda e: e.matmul(pbank[0][:, 0:36], lhsT=h2T[:, k, t * 128:(t + 1) * 128], rhs=wr[:, k, :],
                                                       start=(k == 0), stop=(k == 7)), reads=[d_h2[t], d_wr], writes=[d_pb[0]], inc=(k == 7))
                    R = [d_rt]
                    fw.op("dve", lambda e: e.tensor_tensor(out=lg[:], in0=pbank[0][:, 0:36], in1=brb[:], op=ALU.add),
                          reads=[d_pb[0], d_wr], writes=R)
                    gmax, ngmax, gsum, gw = rt[:, 0:1], rt[:, 1:2], rt[:, 2:3], rt[:, 3:4]
                    gm, ge = rt[:, 4:8], rt[:, 8:12]
                    eig, top8 = rt[:, 16:24], rt[:, 24:32]
                    tmp32 = rt[:, 32:64]
                    dd, e2, wa, wb_ = rt[:, 64:65], rt[:, 65:66], rt[:, 66:67], rt[:, 67:68]
                    m1, m2 = rt[:, 72:80], rt[:, 80:88]
                    fw.op("dve", lambda e: e.tensor_reduce(out=gmax, in_=lg[:, 0:4], axis=AX.X, op=ALU.max), reads=R, writes=R)
                    fw.op("dve", lambda e: e.tensor_scalar(out=ngmax, in0=gmax, scalar1=-1.0, scalar2=None, op0=ALU.mult), reads=R, writes=R)
                    fw.op("act", lambda e: e.activation(out=ge, in_=lg[:, 0:4], func=AF.Exp, bias=ngmax, scale=1.0), reads=R, writes=R)
                    fw.op("dve", lambda e: e.tensor_reduce(out=gsum, in_=ge, axis=AX.X, op=ALU.add), reads=R, writes=R)
                    fw.op("dve", lambda e: e.reciprocal(out=gw, in_=gsum), reads=R, writes=R)
                    fw.op("dve", lambda e: e.tensor_scalar(out=gm, in0=lg[:, 0:4], scalar1=gmax, scalar2=None, op0=ALU.is_equal), reads=R, writes=R)
                    fw.op("dve", lambda e: e.tensor_tensor(out=tmp32.rearrange("p (g x) -> p g x", x=8),
                                                           in0=lg[:, 4:36].rearrange("p (g x) -> p g x", x=8),
                                                           in1=gm.unsqueeze(2).to_broadcast([128, 4, 8]), op=ALU.mult), reads=R, writes=R)
                    fw.op("dve", lambda e: e.tensor_reduce(out=eig, in_=tmp32.rearrange("p (g x) -> p x g", x=8), axis=AX.X, op=ALU.add),
                          reads=R, writes=R)
                    fw.op("dve", lambda e: e.max(out=top8, in_=eig), reads=R, writes=R)
                    fw.op("dve", lambda e: e.tensor_tensor(out=dd, in0=top8[:, 1:2], in1=top8[:, 0:1], op=ALU.subtract), reads=R, writes=R)
                    fw.op("act", lambda e: e.activation(out=e2, in_=dd, func=AF.Exp), reads=R, writes=R)
                    fw.op("dve", lambda e: e.tensor_scalar_add(out=wa, in0=e2, scalar1=1.0), reads=R, writes=R)
                    fw.op("dve", lambda e: e.reciprocal(out=wa, in_=wa), reads=R, writes=R)
                    fw.op("dve", lambda e: e.tensor_tensor(out=wa, in0=wa, in1=gw, op=ALU.mult), reads=R, writes=R)
                    fw.op("dve", lambda e: e.tensor_tensor(out=wb_, in0=wa, in1=e2, op=ALU.mult), reads=R, writes=R)
                    fw.op("dve", lambda e: e.tensor_scalar(out=m1, in0=eig, scalar1=top8[:, 0:1], scalar2=wa, op0=ALU.is_equal, op1=ALU.mult),
                          reads=R, writes=R)
                    fw.op("dve", lambda e: e.tensor_scalar(out=m2, in0=eig, scalar1=top8[:, 1:2], scalar2=wb_, op0=ALU.is_equal, op1=ALU.mult),
                          reads=R, writes=R)
                    fw.op("dve", lambda e: e.tensor_tensor(out=m1, in0=m1, in1=m2, op=ALU.add), reads=R, writes=R)
                    fw.op("dve", lambda e: e.tensor_tensor(out=comb[:, t, :].rearrange("p (g x) -> p g x", x=8),
                                                           in0=gm.unsqueeze(2).to_broadcast([128, 4, 8]),
                                                           in1=m1.unsqueeze(1).to_broadcast([128, 4, 8]), op=ALU.mult),
                          reads=R, writes=[d_comb[t]])

                def load_expert(e_):
                    i = e_ % 2
                    fw.dma("pool", w13[i][:, :, 0:256], w1[l, e_].rearrange("(k p) n -> p k n", p=128), writes=[d_w13[i]])
                    fw.dma("pool", w13[i][:, :, 256:512], w3[l, e_].rearrange("(k p) n -> p k n", p=128), writes=[d_w13[i]])
                    fw.dma("pool", w2s[i][:, 0, :, :], w2[l, e_].rearrange("(k p) n -> p k n", p=128), writes=[d_w2[i]])
                    if with_ctx:
                        fw.op("pool", lambda e: e.tensor_tensor(out=w2s[i][:, 1, :, :], in0=w2s[i][:, 0, :, :],
                                                                in1=gbc[:, 1, :].unsqueeze(1).to_broadcast([128, 2, D]), op=ALU.mult),
                              reads=[d_w2[i], d_gbc], writes=[d_w2[i]])
                    fw.op("pool", lambda e: e.tensor_tensor(out=w2s[i][:, 0, :, :], in0=w2s[i][:, 0, :, :],
                                                            in1=gbc[:, 0, :].unsqueeze(1).to_broadcast([128, 2, D]), op=ALU.mult),
                          reads=[d_w2[i], d_gbc], writes=[d_w2[i]])

                blocks = ([[0, 1]] if with_ctx else []) + [list(range(TC + 4 * i, TC + 4 * i + 4)) for i in range(4)]
                sa2 = msb("sa2", [128, 2, 512])
                d_sa2 = Dep()
                hidT2 = [msb("hidT2_%d" % i, [128, 2, 512], BF16) for i in range(2)]
                d_hidT2 = [Dep() for _ in range(2)]
                mc = dict(o=0)

                def up(e_, blk, gi, fcs=range(4)):
                    wi = e_ % 2
                    n = len(blk) * 128
                    c0 = blk[0] * 128
                    for fc in fcs:
                        for k in range(8):
                            fw.op("pe", lambda e: e.matmul(pbank[fc][:, 0:n], lhsT=w13[wi][:, k, fc * 128:(fc + 1) * 128],
                                                           rhs=h2T[:, k, c0:c0 + n], start=(k == 0), stop=(k == 7)),
                                  reads=[d_w13[wi]] + [d_h2[t] for t in blk], writes=[d_pb[fc]], inc=(k == 7))

                def evac(e_, blk, gi):
                    n = len(blk) * 128
                    for j in range(2):
                        fw.op("act", lambda e: e.activation(out=sa2[:, j, 0:n], in_=pbank[j][:, 0:n], func=AF.Silu),
                              reads=[d_pb[j]], writes=[d_sa2])
                    for j in range(2):
                        fw.op("dve", lambda e: e.tensor_tensor(out=hidT2[gi][:, j, 0:n], in0=pbank[2 + j][:, 0:n], in1=sa2[:, j, 0:n],
                                                               op=ALU.mult),
                              reads=[d_pb[2 + j], d_sa2], writes=[d_hidT2[gi]])

                def down(e_, blk, gi, tis=None):
                    wi = e_ % 2
                    for ti, t in enumerate(blk):
                        if tis is not None and ti not in tis:
                            continue
                        ver = 1 if t < TC else 0
                        ob = 4 + 2 * (mc["o"] % 2)
                        mc["o"] += 1
                        for half in range(2):
                            for k2 in range(2):
                                fw.op("pe", lambda e: e.matmul(pbank[ob + half][:, :], lhsT=hidT2[gi][:, k2, ti * 128:(ti + 1) * 128],
                                                               rhs=w2s[wi][:, ver, k2, half * 512:(half + 1) * 512],
                                                               start=(k2 == 0), stop=(k2 == 1)),
                                      reads=[d_hidT2[gi], d_w2[wi]], writes=[d_pb[ob + half]], inc=(k2 == 1))
                        for half in range(2):
                            fw.op("dve", lambda e: e.scalar_tensor_tensor(out=x_sb[:, t, half * 512:(half + 1) * 512],
                                                                          in0=pbank[ob + half][:, :], scalar=comb[:, t, e_:e_ + 1],
                                                                          in1=x_sb[:, t, half * 512:(half + 1) * 512],
                                                                          op0=ALU.mult, op1=ALU.add),
                                  reads=[d_pb[ob + half], d_comb[t], xdep[t]], writes=[xdep[t]])

                load_expert(0)
                prev = None
                gidx = 0
                for e_ in range(NE):
                    for bi_, blk in enumerate(blocks):
                        gi = gidx % 2
                        gidx += 1
                        for fc in range(4):
                            up(e_, blk, gi, [fc])
                            if prev is not None:
                                down(*prev, tis=[fc])
                        if bi_ == 0 and e_ + 1 < NE and not ("MOE_NOLOAD" in os.environ and e_ >= 1):
                            load_expert(e_ + 1)
                        evac(e_, blk, gi)
                        prev = (e_, blk, gi)
                down(*prev)
                ln_affine(l, ln2_g, ln2_b, qtiles)
                fw.barrier()
                if dbg == ("moe", l):
                    dbg_dump()
                    return
        for t in range(TC, T):
            fw.dma("sp" if t % 2 == 0 else "act", out[b, (t - TC) * 128:(t - TC + 1) * 128, :], x_sb[:, t, :], reads=[xdep[t]])
    try:
        main_loops()
    except _Stop:
        pass
    fw.final_wait("sp")
    es.close()
    return nc, fw


def _prep_shared(inp):
    f = np.float32
    w_in = np.asarray(inp["w_in"], f)
    Ln = w_in.shape[0]
    blocks = np.zeros((Ln, NPASS_IN, D, WBLK), f)

    def acol(kind, idx):
        if kind == "q":
            return slice(idx * 64, idx * 64 + 64)
        if kind == "k":
            return slice(384 + idx * 64, 384 + idx * 64 + 64)
        return slice(512 + idx * 64, 512 + idx * 64 + 64)

    for j in range(A_KV):
        cols = [acol("q", j * 3 + g) for g in range(3)] + [acol("k", j)] * 3 + [acol("v", j)]
        for i, c in enumerate(cols):
            blocks[:, j, :, i * 64:(i + 1) * 64] = w_in[:, :, c]
    for pi in range(3):
        h0, h1 = 2 * pi, min(2 * pi + 1, B_HEADS - 1)
        cols = []
        for part in range(3):
            for h in (h0, h1):
                cols.append(slice(P_A + part * 320 + h * 64, P_A + part * 320 + h * 64 + 64))
        for i, c in enumerate(cols):
            blocks[:, 2 + pi, :, i * 64:(i + 1) * 64] = w_in[:, :, c]
    blocks[:, 5, :, 0:P_C] = w_in[:, :, P_A + P_B:]
    gA = np.concatenate([np.tile(np.asarray(inp["q_gain_a"], f), (1, 3)), np.tile(np.asarray(inp["k_gain_a"], f), (1, 3))], 1)
    gC = np.concatenate([np.asarray(inp["q_lat_gain"], f), np.asarray(inp["kv_lat_gain"], f)], 1)
    rpb = np.asarray(inp["rpb_b"], f)
    nabv = np.zeros((Ln, B_HEADS, 128, NPAT, 128), f)
    nmask = np.zeros((128, NPAT, 128), f)
    for pid, (dr_idx, dc_idx, msk) in enumerate(_NA_PATS):
        g = rpb[:, :, dr_idx, dc_idx]
        nabv[:, :, :, pid, :] = np.where(msk, g, f(0.0))
        nmask[:, pid, :] = np.where(msk, f(0.0), f(-3750.0))
    cosA, sinA = _rope_tables(HD)
    cosC, sinC = _rope_tables(C_ROPE)
    sh = dict(
        w_mod=np.asarray(inp["w_mod"], f), b_mod=np.asarray(inp["b_mod"], f), w_inb=blocks, gA=gA, gC=gC,
        w_uq=np.asarray(inp["w_uq"], f), w_ukv=np.asarray(inp["w_ukv"], f), w_out=np.asarray(inp["w_out"], f),
        ln1_g=np.asarray(inp["ln1_g"], f), ln1_b=np.asarray(inp["ln1_b"], f),
        ln2_g=np.asarray(inp["ln2_g"], f), ln2_b=np.asarray(inp["ln2_b"], f),
        w_r=np.ascontiguousarray(np.concatenate([np.asarray(inp["w_rg"], f), np.asarray(inp["w_re"], f)], -1)),
        b_r=np.ascontiguousarray(np.concatenate([np.asarray(inp["b_rg"], f), np.asarray(inp["b_re"], f)], -1)),
        w1=np.asarray(inp["w1"], f), w3=np.asarray(inp["w3"], f), w2=np.asarray(inp["w2"], f),
        nab=nabv, namask=nmask, cosA=cosA, sinA=sinA, cosC=cosC, sinC=sinC, ident=np.eye(128, dtype=f),
    )
    return sh


def _core_inputs(inp, sh, core, nb=NB):
    f = np.float32
    b0 = core * nb
    x = np.ascontiguousarray(np.asarray(inp["x"], f)[b0:b0 + nb])
    ctx = np.ascontiguousarray(np.asarray(inp["ctx"], f)[b0:b0 + nb])
    cc = np.concatenate([np.asarray(inp["c"], f)[b0:b0 + nb], np.asarray(inp["c_ctx"], f)[None, :]], 0)
    cT = np.ascontiguousarray(cc.reshape(nb + 1, 8, 128).transpose(2, 1, 0))
    m = dict(sh)
    m.update(x=x, ctx=ctx, cT=cT)
    return m


_CACHE = {}


def kernel(**inputs):
    sh = _prep_shared(inputs)
    if "nc" not in _CACHE:
        _CACHE["nc"] = build_program()[0]
    nc = _CACHE["nc"]
    in_maps = [_core_inputs(inputs, sh, c) for c in range(NCORES)]
    res = run_bass_kernel_spmd(nc, in_maps, core_ids=list(range(NCORES)))
    outs = [np.asarray(r["out"]) for r in res.results]
    return np.concatenate(outs, 0).astype(np.float32)
```

```python
import os
from contextlib import ExitStack
import numpy as np
import concourse.bass as bass
import concourse.mybir as mybir
from concourse.bass_utils import run_bass_kernel_spmd

F32 = mybir.dt.float32
BF16 = mybir.dt.bfloat16
AF = mybir.ActivationFunctionType
ALU = mybir.AluOpType
AX = mybir.AxisListType

D = 1024
SEQ = 2048
CTX = 256
T = 18
TC = 2
NCORES = 8
NB = 4
L = 2
GRID_W = 64
HD = 64
A_HEADS, A_KV, A_G = 6, 2, 3
B_HEADS = 5
C_HEADS = 5
C_QR, C_KVR, C_NOPE, C_ROPE, C_V = 256, 128, 64, 32, 64
P_A, P_B, P_C = 640, 960, 416
NE = 32
DE = 256
ALPHA = float((2 * L) ** 0.25)
EPS = 1e-6
NMOD = 6
WBLK = 448
NPASS_IN = 6


class Dep:
    __slots__ = ("lw", "rd")

    def __init__(self):
        self.lw = None
        self.rd = {}


class Fw:
    def __init__(self, nc, es, ndma=None):
        self.nc = nc
        self.eng = {}
        ndma = ndma or {"sp": 8, "pool": 8, "act": 4}
        for name, h in [("pe", nc.tensor), ("act", nc.scalar), ("dve", nc.vector), ("pool", nc.gpsimd),
                        ("sp", nc.sync)]:
            self.eng[name] = dict(h=h, sem=es.enter_context(nc.semaphore("s_" + name)), count=0, seen={})
        self.dq = {}
        for q, n in ndma.items():
            keys = []
            for i in range(n):
                key = "d_%s%d" % (q, i)
                self.eng[key] = dict(h=None, sem=es.enter_context(nc.semaphore(key)), count=0, seen={})
                keys.append(key)
            self.dq[q] = dict(keys=keys, nxt=0)
        self.nwait = 0
        self.nins = 0
        self.dead = False

    def _waits(self, eng, reads, writes):
        E = self.eng[eng]
        deps = {}
        for d in reads:
            if d.lw is not None:
                deps[d.lw[0]] = max(deps.get(d.lw[0], 0), d.lw[1])
        for d in writes:
            if d.lw is not None and d.lw[0] != eng:
                deps[d.lw[0]] = max(deps.get(d.lw[0], 0), d.lw[1])
            for e, c in d.rd.items():
                if e != eng:
                    deps[e] = max(deps.get(e, 0), c)
        for e, c in deps.items():
            if e == eng and eng == "pe":
                continue
            if E["seen"].get(e, 0) < c:
                E["h"].wait_ge(self.eng[e]["sem"], c)
                E["seen"][e] = c
                self.nwait += 1

    def op(self, eng, fn, reads=(), writes=(), inc=True):
        if self.dead:
            return None
        E = self.eng[eng]
        self._waits(eng, reads, writes)
        ins = fn(E["h"])
        self.nins += 1
        c = E["count"] + 1
        if inc:
            ins.then_inc(E["sem"], 1)
            E["count"] = c
        for d in reads:
            d.rd[eng] = max(d.rd.get(eng, 0), c)
        for d in writes:
            d.lw = (eng, c)
            d.rd = {}
        return ins

    def dma(self, q, out, in_, reads=(), writes=(), **kw):
        if self.dead:
            return None
        E = self.eng[q]
        self._waits(q, reads, writes)
        Q = self.dq[q]
        key = Q["keys"][Q["nxt"] % len(Q["keys"])]
        Q["nxt"] += 1
        Dm = self.eng[key]
        if E["seen"].get(key, 0) < Dm["count"]:
            E["h"].wait_ge(Dm["sem"], Dm["count"])
            E["seen"][key] = Dm["count"]
        if q == "pool":
            kw.setdefault("max_dma_last_dim", 4096)
        ins = E["h"].dma_start(out=out, in_=in_, **kw)
        ins.then_inc(Dm["sem"], 16)
        Dm["count"] += 16
        self.nins += 1
        for d in reads:
            d.rd[key] = Dm["count"]
        for d in writes:
            d.lw = (key, Dm["count"])
            d.rd = {}
        return ins

    def barrier(self):
        if self.dead:
            return
        for name in ("pe", "act", "dve", "pool", "sp"):
            E = self.eng[name]
            for k, K in self.eng.items():
                if k == name:
                    continue
                if E["seen"].get(k, 0) < K["count"]:
                    E["h"].wait_ge(K["sem"], K["count"])
                    E["seen"][k] = K["count"]

    def final_wait(self, eng="sp"):
        E = self.eng[eng]
        for k, K in self.eng.items():
            if k == eng:
                continue
            if K["count"] > 0 and E["seen"].get(k, 0) < K["count"]:
                E["h"].wait_ge(K["sem"], K["count"])
                E["seen"][k] = K["count"]


def _rope_tables(dim):
    n = np.arange(SEQ, dtype=np.int32)
    row = (n // GRID_W).astype(np.float32)
    col = (n % GRID_W).astype(np.float32)
    nf = dim // 4
    inv = (np.float32(10000.0) ** (-np.arange(nf, dtype=np.float32) / np.float32(nf))).astype(np.float32)
    ang = np.concatenate([row[:, None] * inv, col[:, None] * inv], -1).astype(np.float32)
    cos = np.concatenate([np.ones((CTX, dim // 2), np.float32), np.cos(ang).astype(np.float32)], 0)
    sin = np.concatenate([np.zeros((CTX, dim // 2), np.float32), np.sin(ang).astype(np.float32)], 0)

    def lay(a):
        return np.ascontiguousarray(a.reshape(T, 128, dim // 2).transpose(1, 0, 2))

    return lay(cos), lay(sin)


def _na_plan():
    rows = SEQ // GRID_W
    win_r = min(8, rows)
    ntile = rows // 2
    col = np.arange(GRID_W)
    c0 = np.clip(col - 8, 0, GRID_W - 16)
    pats = {}
    plan = []
    pat_list = []
    for tq in range(ntile):
        ent = []
        r0s = [int(np.clip(2 * tq + a - win_r // 2, 0, rows - win_r)) for a in (0, 1)]
        jmin = min(r0s) // 2
        jmax = (max(r0s) + win_r - 1) // 2
        for j in range(jmin, jmax + 1):
            sig = (j - tq, r0s[0] - 2 * tq, r0s[1] - 2 * tq - 1)
            if sig not in pats:
                dr_idx = np.zeros((128, 128), np.int64)
                dc_idx = np.zeros((128, 128), np.int64)
                msk = np.zeros((128, 128), bool)
                for a in (0, 1):
                    r = 2 * tq + a
                    for b in (0, 1):
                        kr = 2 * j + b
                        row_in = (r0s[a] <= kr) and (kr < r0s[a] + win_r)
                        dr = kr - r + 7
                        for c in range(GRID_W):
                            kc = np.arange(GRID_W)
                            cin = (kc >= c0[c]) & (kc < c0[c] + 16)
                            dc = np.clip(kc - c + 15, 0, 30)
                            p = b * 64 + kc
                            q = a * 64 + c
                            msk[p, q] = cin & row_in
                            dr_idx[p, q] = min(max(dr, 0), 14)
                            dc_idx[p, q] = dc
                pats[sig] = len(pat_list)
                pat_list.append((dr_idx, dc_idx, msk))
            ent.append((j, pats[sig]))
        plan.append(ent)
    return plan, pat_list


_NA_PLAN, _NA_PATS = _na_plan()
NPAT = len(_NA_PATS)


def build_program(nb_run=NB, n_layers=L, dbg=None):
    nc = bass.Bass("TRN2", target_bir_lowering=False)
    es = ExitStack()

    def din(name, shape, dt=F32):
        return nc.dram_tensor(name, list(shape), dt, kind="ExternalInput").ap()

    x_in = din("x", [NB, SEQ, D])
    ctx_in = din("ctx", [NB, CTX, D])
    cT_in = din("cT", [128, 8, NB + 1])
    w_mod = din("w_mod", [L, D, NMOD * D])
    b_mod = din("b_mod", [L, NMOD * D])
    w_inb = din("w_inb", [L, NPASS_IN, D, WBLK])
    gA_in = din("gA", [L, 384])
    gC_in = din("gC", [L, 384])
    w_uq = din("w_uq", [L, C_QR, C_HEADS * 96])
    w_ukv = din("w_ukv", [L, C_KVR, C_HEADS * 128])
    w_out = din("w_out", [L, D, D])
    ln1_g = din("ln1_g", [L, D])
    ln1_b = din("ln1_b", [L, D])
    ln2_g = din("ln2_g", [L, D])
    ln2_b = din("ln2_b", [L, D])
    w_r = din("w_r", [L, D, 36])
    b_r = din("b_r", [L, 36])
    w1 = din("w1", [L, NE, D, DE])
    w3 = din("w3", [L, NE, D, DE])
    w2 = din("w2", [L, NE, DE, D])
    nab = din("nab", [L, B_HEADS, 128, NPAT, 128])
    namask = din("namask", [128, NPAT, 128])
    cosA_in = din("cosA", [128, T, 32])
    sinA_in = din("sinA", [128, T, 32])
    cosC_in = din("cosC", [128, T, 16])
    sinC_in = din("sinC", [128, T, 16])
    ident_in = din("ident", [128, 128])
    out = nc.dram_tensor("out", [NB, SEQ, D], F32, kind="ExternalOutput").ap()
    dbgx = nc.dram_tensor("dbgx", [T, 128, D], F32, kind="ExternalOutput").ap() if dbg is not None else None

    class _Stop(Exception):
        pass

    subcnt = [0]

    def sub(k):
        subcnt[0] += 1
        hit = (dbg == ("sub", k)) or (dbg is not None and dbg[0] == "subn" and dbg[1] == subcnt[0])
        if hit and not fw.dead:
            fw.barrier()
            dbg_dump()
            fw.dead = True

    def chk(k):
        if dbg == ("step", k):
            fw.barrier()
            dbg_dump()
            return True
        return False

    def dbg_dump():
        for t in range(T):
            fw.dma("sp", dbgx[t], x_sb[:, t, :], reads=[xdep[t]])
    _sk = dict(kind="ExternalOutput") if "SCR_INT" not in os.environ else {}
    mod_d = nc.dram_tensor("mod_d", [L, NB + 1, NMOD * D], F32, **_sk).ap()
    hT_d = nc.dram_tensor("hT_d", [T, 128, 8 * 128], BF16, **_sk).ap()

    fw = Fw(nc, es)

    def sb(name, shape, dt=F32):
        return es.enter_context(nc.sbuf_tensor(name, list(shape), dt))

    def ps(name, shape, dt=F32):
        return es.enter_context(nc.psum_tensor(name, list(shape), dt))

    x_sb = sb("x_sb", [128, T, D])
    xdep = [Dep() for _ in range(T)]
    ident_f = sb("ident_f", [128, 128])
    ident_b = sb("ident_b", [128, 128], BF16)
    modT = sb("modT", [128, L, NMOD * 8, NB + 1])
    d_modT = Dep()
    d_ident = Dep()
    d_modd = Dep()
    stat = sb("stat", [128, 4, 16])
    d_stat = [Dep() for _ in range(4)]
    zb = [sb("zb%d" % i, [128, D], BF16) for i in range(2)]
    d_zb = [Dep() for _ in range(2)]
    z32 = [sb("z32_%d" % i, [128, D]) for i in range(2)]
    d_z32 = [Dep() for _ in range(2)]
    lnp = sb("lnp", [128, 2, D])
    d_lnp = Dep()
    gbc = sb("gbc", [128, 2, D])
    d_gbc = Dep()

    pbank = [ps("pb%d" % i, [128, 512]) for i in range(8)]
    d_pb = [Dep() for _ in range(8)]

    def bank_bf16(i):
        return pbank[i][:].bitcast(BF16)

    state = dict(stat=0, z=0)

    fw.dma("sp", ident_f[:], ident_in[:, :], writes=[d_ident])
    fw.op("dve", lambda e: e.tensor_copy(out=ident_b[:], in_=ident_f[:]), reads=[d_ident], writes=[d_ident])

    with ExitStack() as pes:
        def psb(name, shape, dt=F32):
            return pes.enter_context(nc.sbuf_tensor(name + "_pre", list(shape), dt))

        PRE = dbg[1] if (dbg is not None and dbg[0] == "pre") else 99
        cT = psb("cT", [128, 8, NB + 1])
        scT = psb("scT", [128, 8, NB + 1])
        tmpc = psb("tmpc", [128, 8, NB + 1])
        wm = [psb("wm%d" % i, [128, 8, 512]) for i in range(2)]
        d_wm = [Dep() for _ in range(2)]
        mrow = psb("mrow", [NB + 1, NMOD * D])
        bmr = psb("bmr", [NB + 1, NMOD * D])
        d_c, d_mrow, d_bmr = Dep(), Dep(), Dep()
        fw.dma("sp", cT[:], cT_in[:, :, :], writes=[d_c])
        fw.op("act", lambda e: e.activation(out=tmpc[:], in_=cT[:], func=AF.Exp, scale=-1.0), reads=[d_c], writes=[d_c])
        fw.op("dve", lambda e: e.tensor_scalar_add(out=tmpc[:], in0=tmpc[:], scalar1=1.0), reads=[d_c], writes=[d_c])
        fw.op("dve", lambda e: e.reciprocal(out=tmpc[:], in_=tmpc[:]), reads=[d_c], writes=[d_c])
        fw.op("dve", lambda e: e.tensor_tensor(out=scT[:], in0=cT[:], in1=tmpc[:], op=ALU.mult), reads=[d_c], writes=[d_c])
        nblk = NMOD * D // 512
        for l in range(n_layers if PRE >= 3 else 0):
            fw.dma("sp", bmr[:], b_mod[l, :].partition_broadcast(NB + 1), writes=[d_bmr])
            for blk in range(nblk):
                i = blk % 2
                fw.dma("sp", wm[i][:], w_mod[l].rearrange("(k p) n -> p k n", p=128)[:, :, blk * 512:(blk + 1) * 512],
                       writes=[d_wm[i]])
                for k in range(8):
                    fw.op("pe", lambda e: e.matmul(pbank[0][0:NB + 1, :], lhsT=scT[:, k, :], rhs=wm[i][:, k, :],
                                                   start=(k == 0), stop=(k == 7)),
                          reads=[d_c, d_wm[i]], writes=[d_pb[0]], inc=(k == 7))
                fw.op("dve", lambda e: e.tensor_tensor(out=mrow[:, blk * 512:(blk + 1) * 512], in0=pbank[0][0:NB + 1, :],
                                                       in1=bmr[:, blk * 512:(blk + 1) * 512], op=ALU.add),
                      reads=[d_pb[0], d_bmr], writes=[d_mrow])
            if PRE < 4:
                continue
            fw.dma("sp", mod_d[l], mrow[:], reads=[d_mrow], writes=[d_modd])
            if PRE < 5:
                continue
            for c in range(NMOD * 8):
                fw.op("pe", lambda e: e.transpose(pbank[1][:, c * 8:c * 8 + NB + 1], mrow[:, c * 128:(c + 1) * 128],
                                                  ident_f[0:NB + 1, 0:NB + 1]),
                      reads=[d_mrow, d_ident], writes=[d_pb[1]], inc=(c == NMOD * 8 - 1))
            fw.op("dve", lambda e: e.tensor_copy(out=modT[:, l, :, :],
                                                 in_=pbank[1][:, 0:NMOD * 8 * 8].rearrange("p (c e) -> p c e", e=8)[:, :, 0:NB + 1]),
                  reads=[d_pb[1]], writes=[d_modT])
            if PRE < 6:
                continue
            for j in (1, 4):
                fw.op("dve", lambda e: e.tensor_scalar_add(out=modT[:, l, j * 8:(j + 1) * 8, :],
                                                           in0=modT[:, l, j * 8:(j + 1) * 8, :], scalar1=1.0),
                      reads=[d_modT], writes=[d_modT])
        fw.barrier()

    def ln_stats(xt_ap, dx, eps_col=0):
        s = state["stat"] % 4
        state["stat"] += 1
        st = stat[:, s, :]
        dep = d_stat[s]
        fw.op("dve", lambda e: e.bn_stats(out=st[:, 0:6], in_=xt_ap[:, 0:512]), reads=[dx], writes=[dep])
        fw.op("dve", lambda e: e.bn_stats(out=st[:, 6:12], in_=xt_ap[:, 512:1024]), reads=[dx], writes=[dep])
        fw.op("dve", lambda e: e.bn_aggr(out=st[:, 12:14], in_=st[:, 0:12]), reads=[dep], writes=[dep])
        fw.op("act", lambda e: e.activation(out=st[:, 14:15], in_=st[:, 13:14], func=AF.Ln, bias=epsb[:, eps_col:eps_col + 1], scale=1.0),
              reads=[dep, d_eps], writes=[dep])
        fw.op("act", lambda e: e.activation(out=st[:, 14:15], in_=st[:, 14:15], func=AF.Exp, scale=-0.5),
              reads=[dep], writes=[dep])
        fw.op("dve", lambda e: e.tensor_scalar(out=st[:, 15:16], in0=st[:, 12:13], scalar1=st[:, 14:15], scalar2=-1.0,
                                                op0=ALU.mult, op1=ALU.mult), reads=[dep], writes=[dep])
        return st[:, 14:15], st[:, 15:16], dep

    epsb = sb("epsb", [128, 4])
    d_eps = Dep()
    fw.op("dve", lambda e: e.memset(epsb[:], EPS), writes=[d_eps])
    fw.op("dve", lambda e: e.memset(epsb[:, 1:2], EPS / (ALPHA * ALPHA)), reads=[d_eps], writes=[d_eps])

    def ln_mod_T(l, b, jsh, jsc, tiles, sink):
        for t in tiles:
            col = NB if t < TC else b
            rstd, nmr, dst = ln_stats(x_sb[:, t, :], xdep[t])
            zi = state["z"] % 2
            state["z"] += 1
            fw.op("act", lambda e: e.activation(out=zb[zi][:], in_=x_sb[:, t, :], func=AF.Identity, scale=rstd, bias=nmr),
                  reads=[xdep[t], dst], writes=[d_zb[zi]])
            trv7 = bank_bf16(7).rearrange("p (c n) -> p c n", n=128)
            trv6 = bank_bf16(6).rearrange("p (c n) -> p c n", n=128)
            for c in (0, 2, 4, 6):
                fw.op("pe", lambda e: e.transpose(trv7[:, c // 2, :], zb[zi][:, c * 128:(c + 1) * 128], ident_b[:]),
                      reads=[d_zb[zi], d_ident], writes=[d_pb[7]], inc=(c == 6))
            for c in (1, 3, 5, 7):
                fw.op("pe", lambda e: e.transpose(trv6[:, c // 2, :], zb[zi][:, c * 128:(c + 1) * 128], ident_b[:]),
                      reads=[d_zb[zi], d_ident], writes=[d_pb[6]], inc=(c == 7))
            dstap, ddep, post = sink(t)
            for c in range(8):
                scl = modT[:, l, jsc * 8 + c, col:col + 1]
                shf = modT[:, l, jsh * 8 + c, col:col + 1]
                if c % 2 == 0:
                    fw.op("act", lambda e: e.activation(out=dstap[:, c, :], in_=trv7[:, c // 2, :], func=AF.Identity,
                                                        scale=scl, bias=shf),
                          reads=[d_pb[7], d_modT], writes=[ddep])
                else:
                    fw.op("dve", lambda e: e.tensor_scalar(out=dstap[:, c, :], in0=trv6[:, c // 2, :], scalar1=scl, scalar2=shf,
                                                           op0=ALU.mult, op1=ALU.add),
                          reads=[d_pb[6], d_modT], writes=[ddep])
            if post is not None:
                post(t)

    def scale_x(tiles):
        for t in tiles:
            fw.op("pool", lambda e: e.tensor_scalar(out=x_sb[:, t, :], in0=x_sb[:, t, :], scalar1=ALPHA, scalar2=None,
                                                    op0=ALU.mult), reads=[xdep[t]], writes=[xdep[t]])

    def ln_affine(l, g_in, b_in, tiles):
        fw.dma("sp", lnp[:, 0, :], g_in[l, :].partition_broadcast(128), writes=[d_lnp])
        fw.dma("sp", lnp[:, 1, :], b_in[l, :].partition_broadcast(128), writes=[d_lnp])
        for t in tiles:
            rstd, nmr, dst = ln_stats(x_sb[:, t, :], xdep[t], eps_col=1)
            zi = state["z"] % 2
            state["z"] += 1
            fw.op("act", lambda e: e.activation(out=z32[zi][:], in_=x_sb[:, t, :], func=AF.Identity, scale=rstd, bias=nmr),
                  reads=[xdep[t], dst], writes=[d_z32[zi]])
            fw.op("dve", lambda e: e.tensor_tensor(out=z32[zi][:], in0=z32[zi][:], in1=lnp[:, 0, :], op=ALU.mult),
                  reads=[d_z32[zi], d_lnp], writes=[d_z32[zi]])
            fw.op("pool", lambda e: e.tensor_tensor(out=x_sb[:, t, :], in0=z32[zi][:], in1=lnp[:, 1, :], op=ALU.add),
                  reads=[d_z32[zi], d_lnp], writes=[xdep[t]])

    def load_gates(l, b, j):
        fw.dma("sp", gbc[:, 0, :], mod_d[l, b, j * D:(j + 1) * D].partition_broadcast(128), reads=[d_modd], writes=[d_gbc])
        fw.dma("sp", gbc[:, 1, :], mod_d[l, NB, j * D:(j + 1) * D].partition_broadcast(128), reads=[d_modd], writes=[d_gbc])
        fw.op("dve", lambda e: e.tensor_scalar(out=gbc[:], in0=gbc[:], scalar1=1.0 / ALPHA, scalar2=None, op0=ALU.mult),
              reads=[d_gbc], writes=[d_gbc])

    def main_loops():
      if dbg is not None and dbg[0] == "pre":
          return
      for b in range(nb_run):
        for t in range(T):
            src = ctx_in[b, t * 128:(t + 1) * 128, :] if t < TC else x_in[b, (t - TC) * 128:(t - TC + 1) * 128, :]
            fw.dma("sp" if t % 2 == 0 else "act", x_sb[:, t, :], src, writes=[xdep[t]])
        if chk(1):
            return
        for l in range(n_layers):
            with_ctx = l < n_layers - 1
            qtiles = list(range(0 if with_ctx else TC, T))
            with ExitStack() as aes:
                def asb(name, shape, dt=F32):
                    return aes.enter_context(nc.sbuf_tensor("%s_a%d_%d" % (name, b, l), list(shape), dt))

                hst = [asb("hst%d" % i, [128, 8, 128], BF16) for i in range(2)]
                d_hst = [Dep() for _ in range(2)]
                d_hTd = [Dep() for _ in range(T)]
                hld = [asb("hld%d" % i, [128, 8, 128], BF16) for i in range(2)]
                d_hld = [Dep() for _ in range(2)]
                wblk = asb("wblk", [128, 8, WBLK], BF16)
                d_wblk = Dep()
                wo = asb("wo", [128, 2, 2, D], BF16)
                d_wo = Dep()
                qkT = asb("qkT", [128, 3, T * 128], BF16)
                d_qkT = Dep()
                vaug = asb("vaug", [128, T, 2, 65], BF16)
                d_v = Dep()
                ypass = asb("ypass", [128, T, 192], BF16)
                d_y = Dep()
                yT = [asb("yT%d" % i, [128, 2, 128], BF16) for i in range(2)]
                d_yT = [Dep() for _ in range(2)]
                pT = [asb("pT%d" % i, [128, 512], BF16) for i in range(3)]
                d_pT = [Dep() for _ in range(3)]
                rcp = [asb("rcp%d" % i, [128, 4]) for i in range(2)]
                d_rcp = [Dep() for _ in range(2)]
                tm = [asb("tm%d" % i, [128, WBLK], BF16) for i in range(2)]
                d_tm = [Dep() for _ in range(2)]
                f1 = asb("f1", [128, 384])
                f2 = asb("f2", [128, 384])
                f3 = asb("f3", [128, 384])
                f4 = asb("f4", [128, 384])
                d_f = Dep()
                d_rp = [Dep() for _ in range(4)]
                ssq = asb("ssq", [128, 8])
                gains = asb("gains", [128, 384])
                d_gains = Dep()
                cosT = asb("cosT", [128, T, 32])
                sinT = asb("sinT", [128, T, 32])
                d_cs = Dep()
                d_lat = Dep()
                d_wh = Dep()
                d_na = Dep()
                d_nas = Dep()
                cnt = dict(h=0, pT=0, sT=0, pv=0, tm=0, yT=0, o=0, rc=0)

                fw.op("pool", lambda e: e.memset(vaug[:], 1.0), writes=[d_v])

                def sink1(t):
                    i = cnt["h"] % 2
                    cnt["h"] += 1

                    def post(t, i=i):
                        fw.dma("sp", hT_d[t].rearrange("p (c n) -> p c n", n=128), hst[i][:], reads=[d_hst[i]], writes=[d_hTd[t]])
                    return hst[i], d_hst[i], post

                ln_mod_T(l, b, 0, 1, range(T), sink1)
                if chk(2):
                    return
                load_gates(l, b, 2)
                if chk(3):
                    return

                def load_wblk(pi):
                    fw.dma("pool", wblk[:], w_inb[l, pi].rearrange("(k p) n -> p k n", p=128), writes=[d_wblk])

                def load_wo(r0, nr):
                    n0 = min(nr, 128)
                    fw.dma("pool", wo[0:n0, 0, 0, :], w_out[l, r0:r0 + n0, :], writes=[d_wo])
                    if nr > 128:
                        fw.dma("pool", wo[0:nr - 128, 0, 1, :], w_out[l, r0 + 128:r0 + nr, :], writes=[d_wo])
                    for kc in range(2 if nr > 128 else 1):
                        n1 = min(nr - kc * 128, 128)
                        if with_ctx:
                            fw.op("pool", lambda e: e.tensor_tensor(out=wo[0:n1, 1, kc, :], in0=wo[0:n1, 0, kc, :],
                                                                    in1=gbc[0:n1, 1, :], op=ALU.mult),
                                  reads=[d_wo, d_gbc], writes=[d_wo])
                        fw.op("pool", lambda e: e.tensor_tensor(out=wo[0:n1, 0, kc, :], in0=wo[0:n1, 0, kc, :],
                                                                in1=gbc[0:n1, 0, :], op=ALU.mult),
                              reads=[d_wo, d_gbc], writes=[d_wo])

                def project(ncols, cb):
                    def mm(t):
                        i = t % 2
                        fw.dma("sp", hld[i][:], hT_d[t].rearrange("p (c n) -> p c n", n=128), reads=[d_hTd[t]], writes=[d_hld[i]])
                        pb = 0 + (t % 2)
                        for k in range(8):
                            fw.op("pe", lambda e: e.matmul(pbank[pb][:, 0:ncols], lhsT=hld[i][:, k, :], rhs=wblk[:, k, 0:ncols],
                                                           start=(k == 0), stop=(k == 7)),
                                  reads=[d_hld[i], d_wblk], writes=[d_pb[pb]], inc=(k == 7))
                        return pb

                    pend = mm(0)
                    for t in range(T):
                        pb = pend
                        if t + 1 < T:
                            pend = mm(t + 1)
                        cb(t, pbank[pb], d_pb[pb])

                def rope(src3, dst3, ng, half, t, cs_half, ddst, cs=None):
                    if cs is not None:
                        cosb, sinb = cs
                    else:
                        cosb = cosT[:, t, 0:cs_half].unsqueeze(1).to_broadcast([128, ng, half])
                        sinb = sinT[:, t, 0:cs_half].unsqueeze(1).to_broadcast([128, ng, half])
                    x1 = src3[:, :, 0:half]
                    x2 = src3[:, :, half:2 * half]
                    a = f3[:, 0:ng * half].rearrange("p (g d) -> p g d", d=half)
                    bq = f3[:, 192:192 + ng * half].rearrange("p (g d) -> p g d", d=half)
                    c = f4[:, 0:ng * half].rearrange("p (g d) -> p g d", d=half)
                    dq = f4[:, 192:192 + ng * half].rearrange("p (g d) -> p g d", d=half)
                    fw.op("dve", lambda e: e.tensor_tensor(out=a, in0=x1, in1=cosb, op=ALU.mult), reads=[d_f, d_cs], writes=[d_rp[0]])
                    fw.op("dve", lambda e: e.tensor_tensor(out=bq, in0=x2, in1=sinb, op=ALU.mult), reads=[d_f, d_cs], writes=[d_rp[1]])
                    fw.op("dve", lambda e: e.tensor_tensor(out=c, in0=x1, in1=sinb, op=ALU.mult), reads=[d_f, d_cs], writes=[d_rp[2]])
                    fw.op("dve", lambda e: e.tensor_tensor(out=dq, in0=x2, in1=cosb, op=ALU.mult), reads=[d_f, d_cs], writes=[d_rp[3]])
                    fw.op("dve", lambda e: e.tensor_tensor(out=dst3[:, :, 0:half], in0=a, in1=bq, op=ALU.subtract),
                          reads=[d_rp[0], d_rp[1]], writes=[ddst])
                    fw.op("dve", lambda e: e.tensor_tensor(out=dst3[:, :, half:2 * half], in0=c, in1=dq, op=ALU.add),
                          reads=[d_rp[2], d_rp[3]], writes=[ddst])

                def rstd_of(ss_ap, n, inv_n):
                    fw.op("act", lambda e: e.activation(out=ss_ap, in_=ss_ap, func=AF.Ln, bias=epsb[:, 0:1], scale=inv_n),
                          reads=[d_f, d_eps], writes=[d_f])
                    fw.op("act", lambda e: e.activation(out=ss_ap, in_=ss_ap, func=AF.Exp, scale=-0.5), reads=[d_f], writes=[d_f])

                def transposes_to(src_tm, src_dep, blocks, dst, dst_dep, t):
                    trv = bank_bf16(7).rearrange("p (c n) -> p c n", n=128)
                    for bi, (c0, ncol, blk) in enumerate(blocks):
                        fw.op("pe", lambda e: e.transpose(trv[0:ncol, bi, :], src_tm[:, c0:c0 + ncol], ident_b[:]),
                              reads=[src_dep, d_ident], writes=[d_pb[7]], inc=(bi == len(blocks) - 1))
                    for bi, (c0, ncol, blk) in enumerate(blocks):
                        p0 = 0
                        fw.op("act", lambda e: e.activation(out=dst[p0:ncol, blk, t * 128:(t + 1) * 128], in_=trv[p0:ncol, bi, :],
                                                            func=AF.Copy),
                              reads=[d_pb[7]], writes=[dst_dep])

                def attention(heads, qblocks, scale):
                    for hd in heads:
                        qp0, K, qblk = hd["q"]
                        kp0, _, kblk = hd["k"]
                        for (qts, kts, bsel) in qblocks:
                            nq = len(qts)
                            n = nq * 128
                            q0 = qts[0] * 128
                            pvb = 4 + (cnt["pv"] % 2)
                            cnt["pv"] += 1
                            pvv = pbank[pvb][:, 0:4 * 65].rearrange("p (j d) -> p j d", d=65)

                            def s_mm(i):
                                kt = kts[i]
                                sb_ = 2 + (cnt["sT"] % 2)
                                cnt["sT"] += 1
                                bias_ap = hd["bias"](bsel, kt) if (hd.get("bias") is not None and bsel is not None) else None
                                fw.op("pe", lambda e: e.matmul(pbank[sb_][:, 0:n], lhsT=qkT[kp0:kp0 + K, kblk, kt * 128:(kt + 1) * 128],
                                                               rhs=qkT[qp0:qp0 + K, qblk, q0:q0 + n], start=True, stop=(bias_ap is None)),
                                      reads=[d_qkT], writes=[d_pb[sb_]], inc=(bias_ap is None))
                                if bias_ap is not None:
                                    fw.op("pe", lambda e: e.matmul(pbank[sb_][:, 0:n], lhsT=ident_b[:], rhs=bias_ap, start=False, stop=True),
                                          reads=[d_na, d_ident], writes=[d_pb[sb_]])
                                return sb_

                            pend = s_mm(0)
                            for i in range(len(kts)):
                                sb_ = pend
                                if i + 1 < len(kts):
                                    pend = s_mm(i + 1)
                                pi = cnt["pT"] % 3
                                cnt["pT"] += 1
                                fw.op("act", lambda e: e.activation(out=pT[pi][:, 0:n], in_=pbank[sb_][:, 0:n], func=AF.Exp, scale=scale),
                                      reads=[d_pb[sb_]], writes=[d_pT[pi]])
                                kt = kts[i]
                                for j in range(nq):
                                    fw.op("pe", lambda e: e.matmul(pvv[:, j, :], lhsT=pT[pi][:, j * 128:(j + 1) * 128],
                                                                   rhs=vaug[:, kt, hd["v"], :], start=(i == 0 and j == 0),
                                                                   stop=(i == len(kts) - 1), skip_group_check=True),
                                          reads=[d_pT[pi], d_v], writes=[d_pb[pvb]], inc=(j == nq - 1))
                            ri = cnt["rc"] % 2
                            cnt["rc"] += 1
                            fw.op("dve", lambda e: e.reciprocal(out=rcp[ri][:, 0:nq], in_=pvv[:, 0:nq, 64]), reads=[d_pb[pvb]],
                                  writes=[d_rcp[ri]])
                            yc = hd["ycol"]
                            fw.op("dve", lambda e: e.tensor_tensor(out=ypass[:, qts[0]:qts[0] + nq, yc:yc + 64], in0=pvv[:, 0:nq, 0:64],
                                                                   in1=rcp[ri][:, 0:nq].unsqueeze(2).to_broadcast([128, nq, 64]),
                                                                   op=ALU.mult),
                                  reads=[d_pb[pvb], d_rcp[ri]], writes=[d_y])

                def out_proj(ncol):
                    chunks = [(0, min(ncol, 128))] + ([(128, ncol - 128)] if ncol > 128 else [])
                    trv = bank_bf16(7).rearrange("p (c n) -> p c n", n=128)
                    tl = list(qtiles)

                    def stage_a(idx):
                        t = tl[idx]
                        yi = cnt["yT"] % 2
                        cnt["yT"] += 1
                        cb = 2 * (idx % 2)
                        for ci, (c0, ncl) in enumerate(chunks):
                            fw.op("pe", lambda e: e.transpose(trv[0:ncl, cb + ci, :], ypass[:, t, c0:c0 + ncl], ident_b[:]),
                                  reads=[d_y, d_ident], writes=[d_pb[7]], inc=(ci == len(chunks) - 1))
                        for ci, (c0, ncl) in enumerate(chunks):
                            fw.op("act", lambda e: e.activation(out=yT[yi][0:ncl, ci, :], in_=trv[0:ncl, cb + ci, :], func=AF.Copy),
                                  reads=[d_pb[7]], writes=[d_yT[yi]])
                        return yi

                    def stage_b(idx, yi):
                        t = tl[idx]
                        ver = 1 if t < TC else 0
                        for half in range(2):
                            ob = 2 * (idx % 2) + half
                            for ci, (c0, ncl) in enumerate(chunks):
                                fw.op("pe", lambda e: e.matmul(pbank[ob][:, :], lhsT=yT[yi][0:ncl, ci, :],
                                                               rhs=wo[0:ncl, ver, ci, half * 512:(half + 1) * 512],
                                                               start=(ci == 0), stop=(ci == len(chunks) - 1)),
                                      reads=[d_yT[yi], d_wo], writes=[d_pb[ob]], inc=(ci == len(chunks) - 1))
                            fw.op("dve", lambda e: e.tensor_tensor(out=x_sb[:, t, half * 512:(half + 1) * 512],
                                                                   in0=x_sb[:, t, half * 512:(half + 1) * 512], in1=pbank[ob][:, :],
                                                                   op=ALU.add),
                                  reads=[d_pb[ob], xdep[t]], writes=[xdep[t]])

                    pend = stage_a(0)
                    for idx in range(len(tl)):
                        yi = pend
                        if idx + 1 < len(tl):
                            pend = stage_a(idx + 1)
                        stage_b(idx, yi)

                lat_blocks = [(list(range(TC + 4 * i, TC + 4 * i + 4)), list(range(T)), None) for i in range(4)]
                ctx_blocks = [([0, 1], [0, 1], None)] if with_ctx else []

                fw.dma("sp", cosT[:], cosA_in[:, :, :], writes=[d_cs])
                fw.dma("sp", sinT[:], sinA_in[:, :, :], writes=[d_cs])
                fw.dma("sp", gains[:], gA_in[l, :].partition_broadcast(128), writes=[d_gains])
                if chk(4):
                    return
                for j in range(A_KV):
                    load_wblk(j)
                    if chk(5):
                        return
                    load_wo(j * 192, 192)
                    if chk(6):
                        return

                    def cbA(t, pb, dpb):
                        ti = cnt["tm"] % 2
                        cnt["tm"] += 1
                        parts = os.environ.get("CB_PARTS", "1234")
                        if "1" in parts:
                            if "NOSQ" in os.environ:
                                fw.op("act", lambda e: e.activation(out=f2[:], in_=pb[:, 0:384], func=AF.Copy), reads=[dpb], writes=[d_f])
                                fw.op("dve", lambda e: e.tensor_tensor(out=f1[:], in0=f2[:], in1=f2[:], op=ALU.mult), reads=[d_f], writes=[d_f])
                            else:
                                fw.op("act", lambda e: e.activation(out=f1[:], in_=pb[:, 0:384], func=AF.Square), reads=[dpb], writes=[d_f])
                            fw.op("dve", lambda e: e.tensor_reduce(out=ssq[:, 0:6], in_=f1[:].rearrange("p (g d) -> p g d", d=64),
                                                                   axis=AX.X, op=ALU.add), reads=[d_f], writes=[d_f])
                            rstd_of(ssq[:, 0:6], 6, 1.0 / 64)
                            fw.op("dve", lambda e: e.tensor_tensor(out=f2[:].rearrange("p (g d) -> p g d", d=64),
                                                                   in0=pb[:, 0:384].rearrange("p (g d) -> p g d", d=64),
                                                                   in1=ssq[:, 0:6].unsqueeze(2).to_broadcast([128, 6, 64]), op=ALU.mult),
                                  reads=[dpb, d_f], writes=[d_f])
                            fw.op("dve", lambda e: e.tensor_tensor(out=f2[:], in0=f2[:], in1=gains[:], op=ALU.mult),
                                  reads=[d_f, d_gains], writes=[d_f])
                        if "2" in parts:
                            rope(f2[:].rearrange("p (g d) -> p g d", d=64), tm[ti][:, 0:384].rearrange("p (g d) -> p g d", d=64), 6, 32, t, 32, d_tm[ti])
                        if "3" in parts:
                            if os.environ.get("V_ENG", "dve") == "act":
                                fw.op("act", lambda e: e.activation(out=vaug[:, t, 0, 0:64], in_=pb[:, 384:448], func=AF.Copy),
                                      reads=[dpb], writes=[d_v])
                            else:
                                fw.op("dve", lambda e: e.tensor_copy(out=vaug[:, t, 0, 0:64], in_=pb[:, 384:448]),
                                      reads=[dpb], writes=[d_v])
                        if "4" in parts:
                            transposes_to(tm[ti], d_tm[ti], [(0, 128, 0), (128, 128, 1), (256, 128, 2)], qkT, d_qkT, t)

                    project(448, cbA)
                    if chk(7):
                        return
                    heads = [dict(q=((g % 2) * 64, 64, g // 2), k=((g % 2) * 64, 64, 2), v=0, ycol=g * 64) for g in range(A_G)]
                    attention(heads, ctx_blocks + lat_blocks, HD ** -0.5)
                    if chk(8):
                        return
                    out_proj(192)
                    if chk(9):
                        return

                if dbg == ("A", l):
                    fw.barrier()
                    dbg_dump()
                    return
                fw.barrier()
                with ExitStack() as bes:
                    natab = bes.enter_context(nc.sbuf_tensor("natab_%d_%d" % (b, l), [128, 2, NPAT, 128], BF16))
                    nmaskb = bes.enter_context(nc.sbuf_tensor("nmaskb_%d_%d" % (b, l), [128, NPAT, 128], BF16))
                    fw.dma("pool", nmaskb[:], namask[:, :, :], writes=[d_nas])
                    for pi in range(3):
                        h0 = 2 * pi
                        h1 = min(2 * pi + 1, B_HEADS - 1)
                        nh = 2 if pi < 2 else 1
                        load_wblk(2 + pi)
                        for hh, hx in enumerate((h0, h1)[:nh]):
                            fw.dma("pool", natab[:, hh, :, :], nab[l, hx], writes=[d_na])
                            fw.op("pool", lambda e: e.tensor_tensor(out=natab[:, hh, :, :], in0=natab[:, hh, :, :], in1=nmaskb[:], op=ALU.add),
                                  reads=[d_nas, d_na], writes=[d_na])
                        load_wo(384 + h0 * 64, nh * 64)

                        def cbB(t, pb, dpb):
                            ti = cnt["tm"] % 2
                            cnt["tm"] += 1
                            fw.op("dve", lambda e: e.tensor_scalar(out=tm[ti][:, 0:128], in0=pb[:, 0:128], scalar1=float(HD ** -0.5),
                                                                   scalar2=None, op0=ALU.mult), reads=[dpb], writes=[d_tm[ti]])
                            fw.op("dve", lambda e: e.tensor_copy(out=tm[ti][:, 128:256], in_=pb[:, 128:256]), reads=[dpb],
                                  writes=[d_tm[ti]])
                            fw.op("dve", lambda e: e.tensor_copy(out=vaug[:, t, :, 0:64], in_=pb[:, 256:384].rearrange("p (g d) -> p g d", d=64)),
                                  reads=[dpb], writes=[d_v])
                            transposes_to(tm[ti], d_tm[ti], [(0, 128, 0), (128, 128, 1)], qkT, d_qkT, t)

                        project(384, cbB)
                        heads = []
                        for hh in range(nh):
                            heads.append(dict(q=(hh * 64, 64, 0), k=(hh * 64, 64, 1), v=hh, ycol=hh * 64,
                                              bias=(lambda bsel, kt, hh=hh: natab[:, hh, bsel[kt], :] if kt in bsel else None)))
                        qb = list(ctx_blocks)
                        for tq in range(T - TC):
                            ent = _NA_PLAN[tq]
                            kts = [TC + jj for (jj, pid) in ent] + [0, 1]
                            bsel = {TC + jj: pid for (jj, pid) in ent}
                            qb.append(([TC + tq], kts, bsel))
                        attention(heads, qb, 1.0)
                        out_proj(nh * 64)
                    fw.barrier()

                if dbg == ("B", l):
                    dbg_dump()
                    return
                with ExitStack() as ces:
                    latT = ces.enter_context(nc.sbuf_tensor("latT_%d_%d" % (b, l), [128, 3, T * 128], BF16))
                    wq_h = ces.enter_context(nc.sbuf_tensor("wq_h_%d_%d" % (b, l), [128, 2, 96], BF16))
                    wkv_h = ces.enter_context(nc.sbuf_tensor("wkv_h_%d_%d" % (b, l), [128, 128], BF16))
                    fw.dma("sp", cosT[:, :, 0:16], cosC_in[:, :, :], writes=[d_cs])
                    fw.dma("sp", sinT[:, :, 0:16], sinC_in[:, :, :], writes=[d_cs])
                    fw.dma("sp", gains[:], gC_in[l, :].partition_broadcast(128), writes=[d_gains])
                    load_wblk(5)

                    def cbC(t, pb, dpb):
                        ti = cnt["tm"] % 2
                        cnt["tm"] += 1
                        fw.op("act", lambda e: e.activation(out=f1[:], in_=pb[:, 0:384], func=AF.Square), reads=[dpb], writes=[d_f])
                        fw.op("dve", lambda e: e.tensor_reduce(out=ssq[:, 0:1], in_=f1[:, 0:256], axis=AX.X, op=ALU.add), reads=[d_f], writes=[d_f])
                        fw.op("dve", lambda e: e.tensor_reduce(out=ssq[:, 1:2], in_=f1[:, 256:384], axis=AX.X, op=ALU.add), reads=[d_f], writes=[d_f])
                        rstd_of(ssq[:, 0:1], 1, 1.0 / 256)
                        rstd_of(ssq[:, 1:2], 1, 1.0 / 128)
                        fw.op("dve", lambda e: e.tensor_scalar(out=f2[:, 0:256], in0=pb[:, 0:256], scalar1=ssq[:, 0:1], scalar2=None, op0=ALU.mult),
                              reads=[dpb, d_f], writes=[d_f])
                        fw.op("dve", lambda e: e.tensor_scalar(out=f2[:, 256:384], in0=pb[:, 256:384], scalar1=ssq[:, 1:2], scalar2=None, op0=ALU.mult),
                              reads=[dpb, d_f], writes=[d_f])
                        fw.op("pool", lambda e: e.tensor_tensor(out=tm[ti][:, 0:384], in0=f2[:], in1=gains[:], op=ALU.mult),
                              reads=[d_f, d_gains], writes=[d_tm[ti]])
                        fw.op("dve", lambda e: e.tensor_copy(out=f1[:, 0:32], in_=pb[:, 384:416]), reads=[dpb], writes=[d_f])
                        rope(f1[:, 0:32].rearrange("p (g d) -> p g d", d=32), tm[ti][:, 384:416].rearrange("p (g d) -> p g d", d=32), 1, 16, t, 16, d_tm[ti])
                        transposes_to(tm[ti], d_tm[ti], [(0, 128, 0), (128, 128, 1), (256, 128, 2)], latT, d_lat, t)
                        trv = bank_bf16(7).rearrange("p (c n) -> p c n", n=128)
                        fw.op("pe", lambda e: e.transpose(trv[0:96, 3, :], tm[ti][:, 320:416], ident_b[:]),
                              reads=[d_tm[ti], d_ident], writes=[d_pb[7]])
                        fw.op("act", lambda e: e.activation(out=qkT[64:96, 1, t * 128:(t + 1) * 128], in_=trv[64:96, 3, :], func=AF.Copy),
                              reads=[d_pb[7]], writes=[d_qkT])

                    project(416, cbC)
                    for h in range(C_HEADS):
                        fw.dma("pool", wq_h[:], w_uq[l].rearrange("(k p) n -> p k n", p=128)[:, :, h * 96:(h + 1) * 96], writes=[d_wh])
                        fw.dma("pool", wkv_h[:], w_ukv[l, :, h * 128:(h + 1) * 128], writes=[d_wh])
                        if h == 0:
                            load_wo(704, 192)
                        if h == 3:
                            load_wo(704 + 192, 128)
                        G = 3

                        def mmC2(p):
                            pb = p % 2
                            for s_ in range(G):
                                t = G * p + s_
                                o = s_ * 160
                                for kc in range(2):
                                    fw.op("pe", lambda e: e.matmul(pbank[pb][:, o:o + 96], lhsT=latT[:, kc, t * 128:(t + 1) * 128],
                                                                   rhs=wq_h[:, kc, :], start=(kc == 0), stop=(kc == 1)),
                                          reads=[d_lat, d_wh], writes=[d_pb[pb]], inc=False)
                                fw.op("pe", lambda e: e.matmul(pbank[pb][:, o + 96:o + 160], lhsT=latT[:, 2, t * 128:(t + 1) * 128],
                                                               rhs=wkv_h[:, 64:128], start=False, stop=True, skip_group_check=True),
                                      reads=[d_lat, d_wh], writes=[d_pb[pb]], inc=(s_ == G - 1))
                            return pb

                        pendC = mmC2(0)
                        for p in range(T // G):
                            ti = cnt["tm"] % 2
                            cnt["tm"] += 1
                            pb = pendC
                            if p + 1 < T // G:
                                pendC = mmC2(p + 1)
                            t0 = G * p
                            pv = pbank[pb][:, 0:G * 160].rearrange("p (s c) -> p s c", c=160)
                            tmv = tm[ti][:, 0:G * 96].rearrange("p (s c) -> p s c", c=96)
                            fw.op("act", lambda e: e.activation(out=tmv[:, :, 0:64], in_=pv[:, :, 0:64], func=AF.Copy), reads=[d_pb[pb]],
                                  writes=[d_tm[ti]])
                            fw.op("act", lambda e: e.activation(out=vaug[:, t0:t0 + G, 0, 0:64], in_=pv[:, :, 96:160], func=AF.Copy),
                                  reads=[d_pb[pb]], writes=[d_v])
                            fw.op("act", lambda e: e.activation(out=f1[:, 0:G * 32].rearrange("p (s c) -> p s c", c=32), in_=pv[:, :, 64:96],
                                                                func=AF.Copy), reads=[d_pb[pb]], writes=[d_f])
                            rope(f1[:, 0:G * 32].rearrange("p (g d) -> p g d", d=32), tmv[:, :, 64:96], G, 16, t0, 16, d_tm[ti],
                                 cs=(cosT[:, t0:t0 + G, 0:16], sinT[:, t0:t0 + G, 0:16]))
                            trv = bank_bf16(7).rearrange("p (c n) -> p c n", n=128)
                            for s_ in range(G):
                                fw.op("pe", lambda e: e.transpose(trv[0:96, s_, :], tm[ti][:, s_ * 96:(s_ + 1) * 96], ident_b[:]),
                                      reads=[d_tm[ti], d_ident], writes=[d_pb[7]], inc=(s_ == G - 1))
                            fw.op("act", lambda e: e.activation(out=qkT[0:96, 0, t0 * 128:(t0 + G) * 128].rearrange("p (s n) -> p s n", n=128),
                                                                in_=trv[0:96, 0:G, :], func=AF.Copy),
                                  reads=[d_pb[7]], writes=[d_qkT])
                        for blk in range(0, T * 128, 512):
                            nn = min(512, T * 128 - blk)
                            sbk = 2 + ((blk // 512) % 2)
                            fw.op("pe", lambda e: e.matmul(pbank[sbk][0:64, 0:nn], lhsT=wkv_h[:, 0:64], rhs=latT[:, 2, blk:blk + nn], start=True, stop=True),
                                  reads=[d_lat, d_wh], writes=[d_pb[sbk]])
                            fw.op("act", lambda e: e.activation(out=qkT[0:64, 1, blk:blk + nn], in_=pbank[sbk][0:64, 0:nn], func=AF.Copy),
                                  reads=[d_pb[sbk]], writes=[d_qkT])
                        heads = [dict(q=(0, 96, 0), k=(0, 96, 1), v=0, ycol=(h % 3) * 64)]
                        attention(heads, ctx_blocks + lat_blocks, (C_NOPE + C_ROPE) ** -0.5)
                        if h == 2:
                            out_proj(192)
                        if h == 4:
                            out_proj(128)

                ln_affine(l, ln1_g, ln1_b, qtiles)
                fw.barrier()
                if dbg == ("attn", l):
                    dbg_dump()
                    return

            with ExitStack() as mes:
                def msb(name, shape, dt=F32):
                    return mes.enter_context(nc.sbuf_tensor("%s_m%d_%d" % (name, b, l), list(shape), dt))

                h2T = msb("h2T", [128, 8, T * 128], BF16)
                d_h2 = [Dep() for _ in range(T)]
                w13 = [msb("w13_%d" % i, [128, 8, 512], BF16) for i in range(2)]
                w2s = [msb("w2s_%d" % i, [128, 2, 2, D], BF16) for i in range(2)]
                d_w13 = [Dep() for _ in range(2)]
                d_w2 = [Dep() for _ in range(2)]
                wr = msb("wr", [128, 8, 36], BF16)
                brb = msb("brb", [128, 36])
                d_wr = Dep()
                comb = msb("comb", [128, T, NE])
                d_comb = [Dep() for _ in range(T)]
                lg = msb("lg", [128, 36])
                rt = msb("rt", [128, 96])
                d_rt = Dep()

                def sink2(t):
                    return h2T[:, :, t * 128:(t + 1) * 128], d_h2[t], None

                ln_mod_T(l, b, 3, 4, qtiles, sink2)
                load_gates(l, b, 5)
                fw.dma("pool", wr[:], w_r[l].rearrange("(k p) n -> p k n", p=128), writes=[d_wr])
                fw.dma("sp", brb[:], b_r[l, :].partition_broadcast(128), writes=[d_wr])
                for t in qtiles:
                    for k in range(8):
                        fw.op("pe", lambda e: e.matmul(pbank[0][:, 0:36], lhsT=h2T[:, k, t * 128:(t + 1) * 128], rhs=wr[:, k, :],
                                                       start=(k == 0), stop=(k == 7)), reads=[d_h2[t], d_wr], writes=[d_pb[0]], inc=(k == 7))
                    R = [d_rt]
                    fw.op("dve", lambda e: e.tensor_tensor(out=lg[:], in0=pbank[0][:, 0:36], in1=brb[:], op=ALU.add),
                          reads=[d_pb[0], d_wr], writes=R)
                    gmax, ngmax, gsum, gw = rt[:, 0:1], rt[:, 1:2], rt[:, 2:3], rt[:, 3:4]
                    gm, ge = rt[:, 4:8], rt[:, 8:12]
                    eig, top8 = rt[:, 16:24], rt[:, 24:32]
                    tmp32 = rt[:, 32:64]
                    dd, e2, wa, wb_ = rt[:, 64:65], rt[:, 65:66], rt[:, 66:67], rt[:, 67:68]
                    m1, m2 = rt[:, 72:80], rt[:, 80:88]
                    fw.op("dve", lambda e: e.tensor_reduce(out=gmax, in_=lg[:, 0:4], axis=AX.X, op=ALU.max), reads=R, writes=R)
                    fw.op("dve", lambda e: e.tensor_scalar(out=ngmax, in0=gmax, scalar1=-1.0, scalar2=None, op0=ALU.mult), reads=R, writes=R)
                    fw.op("act", lambda e: e.activation(out=ge, in_=lg[:, 0:4], func=AF.Exp, bias=ngmax, scale=1.0), reads=R, writes=R)
                    fw.op("dve", lambda e: e.tensor_reduce(out=gsum, in_=ge, axis=AX.X, op=ALU.add), reads=R, writes=R)
                    fw.op("dve", lambda e: e.reciprocal(out=gw, in_=gsum), reads=R, writes=R)
                    fw.op("dve", lambda e: e.tensor_scalar(out=gm, in0=lg[:, 0:4], scalar1=gmax, scalar2=None, op0=ALU.is_equal), reads=R, writes=R)
                    fw.op("dve", lambda e: e.tensor_tensor(out=tmp32.rearrange("p (g x) -> p g x", x=8),
                                                           in0=lg[:, 4:36].rearrange("p (g x) -> p g x", x=8),
                                                           in1=gm.unsqueeze(2).to_broadcast([128, 4, 8]), op=ALU.mult), reads=R, writes=R)
                    fw.op("dve", lambda e: e.tensor_reduce(out=eig, in_=tmp32.rearrange("p (g x) -> p x g", x=8), axis=AX.X, op=ALU.add),
                          reads=R, writes=R)
                    fw.op("dve", lambda e: e.max(out=top8, in_=eig), reads=R, writes=R)
                    fw.op("dve", lambda e: e.tensor_tensor(out=dd, in0=top8[:, 1:2], in1=top8[:, 0:1], op=ALU.subtract), reads=R, writes=R)
                    fw.op("act", lambda e: e.activation(out=e2, in_=dd, func=AF.Exp), reads=R, writes=R)
                    fw.op("dve", lambda e: e.tensor_scalar_add(out=wa, in0=e2, scalar1=1.0), reads=R, writes=R)
                    fw.op("dve", lambda e: e.reciprocal(out=wa, in_=wa), reads=R, writes=R)
                    fw.op("dve", lambda e: e.tensor_tensor(out=wa, in0=wa, in1=gw, op=ALU.mult), reads=R, writes=R)
                    fw.op("dve", lambda e: e.tensor_tensor(out=wb_, in0=wa, in1=e2, op=ALU.mult), reads=R, writes=R)
                    fw.op("dve", lambda e: e.tensor_scalar(out=m1, in0=eig, scalar1=top8[:, 0:1], scalar2=wa, op0=ALU.is_equal, op1=ALU.mult),
                          reads=R, writes=R)
                    fw.op("dve", lambda e: e.tensor_scalar(out=m2, in0=eig, scalar1=top8[:, 1:2], scalar2=wb_, op0=ALU.is_equal, op1=ALU.mult),
                          reads=R, writes=R)
                    fw.op("dve", lambda e: e.tensor_tensor(out=m1, in0=m1, in1=m2, op=ALU.add), reads=R, writes=R)
                    fw.op("dve", lambda e: e.tensor_tensor(out=comb[:, t, :].rearrange("p (g x) -> p g x", x=8),
                                                           in0=gm.unsqueeze(2).to_broadcast([128, 4, 8]),
                                                           in1=m1.unsqueeze(1).to_broadcast([128, 4, 8]), op=ALU.mult),
                          reads=R, writes=[d_comb[t]])

                def load_expert(e_):
                    i = e_ % 2
                    fw.dma("pool", w13[i][:, :, 0:256], w1[l, e_].rearrange("(k p) n -> p k n", p=128), writes=[d_w13[i]])
                    fw.dma("pool", w13[i][:, :, 256:512], w3[l, e_].rearrange("(k p) n -> p k n", p=128), writes=[d_w13[i]])
                    fw.dma("pool", w2s[i][:, 0, :, :], w2[l, e_].rearrange("(k p) n -> p k n", p=128), writes=[d_w2[i]])
                    if with_ctx:
                        fw.op("pool", lambda e: e.tensor_tensor(out=w2s[i][:, 1, :, :], in0=w2s[i][:, 0, :, :],
                                                                in1=gbc[:, 1, :].unsqueeze(1).to_broadcast([128, 2, D]), op=ALU.mult),
                              reads=[d_w2[i], d_gbc], writes=[d_w2[i]])
                    fw.op("pool", lambda e: e.tensor_tensor(out=w2s[i][:, 0, :, :], in0=w2s[i][:, 0, :, :],
                                                            in1=gbc[:, 0, :].unsqueeze(1).to_broadcast([128, 2, D]), op=ALU.mult),
                          reads=[d_w2[i], d_gbc], writes=[d_w2[i]])

                blocks = ([[0, 1]] if with_ctx else []) + [list(range(TC + 4 * i, TC + 4 * i + 4)) for i in range(4)]
                sa2 = msb("sa2", [128, 2, 512])
                d_sa2 = Dep()
                hidT2 = [msb("hidT2_%d" % i, [128, 2, 512], BF16) for i in range(2)]
                d_hidT2 = [Dep() for _ in range(2)]
                mc = dict(o=0)

                def up(e_, blk, gi, fcs=range(4)):
                    wi = e_ % 2
                    n = len(blk) * 128
                    c0 = blk[0] * 128
                    for fc in fcs:
                        for k in range(8):
                            fw.op("pe", lambda e: e.matmul(pbank[fc][:, 0:n], lhsT=w13[wi][:, k, fc * 128:(fc + 1) * 128],
                                                           rhs=h2T[:, k, c0:c0 + n], start=(k == 0), stop=(k == 7)),
                                  reads=[d_w13[wi]] + [d_h2[t] for t in blk], writes=[d_pb[fc]], inc=(k == 7))

                def evac(e_, blk, gi):
                    n = len(blk) * 128
                    for j in range(2):
                        fw.op("act", lambda e: e.activation(out=sa2[:, j, 0:n], in_=pbank[j][:, 0:n], func=AF.Silu),
                              reads=[d_pb[j]], writes=[d_sa2])
                    for j in range(2):
                        fw.op("dve", lambda e: e.tensor_tensor(out=hidT2[gi][:, j, 0:n], in0=pbank[2 + j][:, 0:n], in1=sa2[:, j, 0:n],
                                                               op=ALU.mult),
                              reads=[d_pb[2 + j], d_sa2], writes=[d_hidT2[gi]])

                def down(e_, blk, gi, tis=None):
                    wi = e_ % 2
                    for ti, t in enumerate(blk):
                        if tis is not None and ti not in tis:
                            continue
                        ver = 1 if t < TC else 0
                        ob = 4 + 2 * (mc["o"] % 2)
                        mc["o"] += 1
                        for half in range(2):
                            for k2 in range(2):
                                fw.op("pe", lambda e: e.matmul(pbank[ob + half][:, :], lhsT=hidT2[gi][:, k2, ti * 128:(ti + 1) * 128],
                                                               rhs=w2s[wi][:, ver, k2, half * 512:(half + 1) * 512],
                                                               start=(k2 == 0), stop=(k2 == 1)),
                                      reads=[d_hidT2[gi], d_w2[wi]], writes=[d_pb[ob + half]], inc=(k2 == 1))
                        for half in range(2):
                            fw.op("dve", lambda e: e.scalar_tensor_tensor(out=x_sb[:, t, half * 512:(half + 1) * 512],
                                                                          in0=pbank[ob + half][:, :], scalar=comb[:, t, e_:e_ + 1],
                                                                          in1=x_sb[:, t, half * 512:(half + 1) * 512],
                                                                          op0=ALU.mult, op1=ALU.add),
                                  reads=[d_pb[ob + half], d_comb[t], xdep[t]], writes=[xdep[t]])

                load_expert(0)
                prev = None
                gidx = 0
                for e_ in range(NE):
                    for bi_, blk in enumerate(blocks):
                        gi = gidx % 2
                        gidx += 1
                        for fc in range(4):
                            up(e_, blk, gi, [fc])
                            if prev is not None:
                                down(*prev, tis=[fc])
                        if bi_ == 0 and e_ + 1 < NE and not ("MOE_NOLOAD" in os.environ and e_ >= 1):
                            load_expert(e_ + 1)
                        evac(e_, blk, gi)
                        prev = (e_, blk, gi)
                down(*prev)
                ln_affine(l, ln2_g, ln2_b, qtiles)
                fw.barrier()
                if dbg == ("moe", l):
                    dbg_dump()
                    return
        for t in range(TC, T):
            fw.dma("sp" if t % 2 == 0 else "act", out[b, (t - TC) * 128:(t - TC + 1) * 128, :], x_sb[:, t, :], reads=[xdep[t]])
    try:
        main_loops()
    except _Stop:
        pass
    fw.final_wait("sp")
    es.close()
    return nc, fw


def _prep_shared(inp):
    f = np.float32
    w_in = np.asarray(inp["w_in"], f)
    Ln = w_in.shape[0]
    blocks = np.zeros((Ln, NPASS_IN, D, WBLK), f)

    def acol(kind, idx):
        if kind == "q":
            return slice(idx * 64, idx * 64 + 64)
        if kind == "k":
            return slice(384 + idx * 64, 384 + idx * 64 + 64)
        return slice(512 + idx * 64, 512 + idx * 64 + 64)

    for j in range(A_KV):
        cols = [acol("q", j * 3 + g) for g in range(3)] + [acol("k", j)] * 3 + [acol("v", j)]
        for i, c in enumerate(cols):
            blocks[:, j, :, i * 64:(i + 1) * 64] = w_in[:, :, c]
    for pi in range(3):
        h0, h1 = 2 * pi, min(2 * pi + 1, B_HEADS - 1)
        cols = []
        for part in range(3):
            for h in (h0, h1):
                cols.append(slice(P_A + part * 320 + h * 64, P_A + part * 320 + h * 64 + 64))
        for i, c in enumerate(cols):
            blocks[:, 2 + pi, :, i * 64:(i + 1) * 64] = w_in[:, :, c]
    blocks[:, 5, :, 0:P_C] = w_in[:, :, P_A + P_B:]
    gA = np.concatenate([np.tile(np.asarray(inp["q_gain_a"], f), (1, 3)), np.tile(np.asarray(inp["k_gain_a"], f), (1, 3))], 1)
    gC = np.concatenate([np.asarray(inp["q_lat_gain"], f), np.asarray(inp["kv_lat_gain"], f)], 1)
    rpb = np.asarray(inp["rpb_b"], f)
    nabv = np.zeros((Ln, B_HEADS, 128, NPAT, 128), f)
    nmask = np.zeros((128, NPAT, 128), f)
    for pid, (dr_idx, dc_idx, msk) in enumerate(_NA_PATS):
        g = rpb[:, :, dr_idx, dc_idx]
        nabv[:, :, :, pid, :] = np.where(msk, g, f(0.0))
        nmask[:, pid, :] = np.where(msk, f(0.0), f(-3750.0))
    cosA, sinA = _rope_tables(HD)
    cosC, sinC = _rope_tables(C_ROPE)
    sh = dict(
        w_mod=np.asarray(inp["w_mod"], f), b_mod=np.asarray(inp["b_mod"], f), w_inb=blocks, gA=gA, gC=gC,
        w_uq=np.asarray(inp["w_uq"], f), w_ukv=np.asarray(inp["w_ukv"], f), w_out=np.asarray(inp["w_out"], f),
        ln1_g=np.asarray(inp["ln1_g"], f), ln1_b=np.asarray(inp["ln1_b"], f),
        ln2_g=np.asarray(inp["ln2_g"], f), ln2_b=np.asarray(inp["ln2_b"], f),
        w_r=np.ascontiguousarray(np.concatenate([np.asarray(inp["w_rg"], f), np.asarray(inp["w_re"], f)], -1)),
        b_r=np.ascontiguousarray(np.concatenate([np.asarray(inp["b_rg"], f), np.asarray(inp["b_re"], f)], -1)),
        w1=np.asarray(inp["w1"], f), w3=np.asarray(inp["w3"], f), w2=np.asarray(inp["w2"], f),
        nab=nabv, namask=nmask, cosA=cosA, sinA=sinA, cosC=cosC, sinC=sinC, ident=np.eye(128, dtype=f),
    )
    return sh


def _core_inputs(inp, sh, core, nb=NB):
    f = np.float32
    b0 = core * nb
    x = np.ascontiguousarray(np.asarray(inp["x"], f)[b0:b0 + nb])
    ctx = np.ascontiguousarray(np.asarray(inp["ctx"], f)[b0:b0 + nb])
    cc = np.concatenate([np.asarray(inp["c"], f)[b0:b0 + nb], np.asarray(inp["c_ctx"], f)[None, :]], 0)
    cT = np.ascontiguousarray(cc.reshape(nb + 1, 8, 128).transpose(2, 1, 0))
    m = dict(sh)
    m.update(x=x, ctx=ctx, cT=cT)
    return m


_CACHE = {}


def kernel(**inputs):
    sh = _prep_shared(inputs)
    if "nc" not in _CACHE:
        _CACHE["nc"] = build_program()[0]
    nc = _CACHE["nc"]
    in_maps = [_core_inputs(inputs, sh, c) for c in range(NCORES)]
    res = run_bass_kernel_spmd(nc, in_maps, core_ids=list(range(NCORES)))
    outs = [np.asarray(r["out"]) for r in res.results]
    return np.concatenate(outs, 0).astype(np.float32)
```
